# Optimizing a Trainium2 kernel written in Bass

```python
import jax, jax.numpy as jnp
from jax import lax
import numpy as np

D_MODEL = 1024
BATCH = 4
SEQ = 8192
DEPTH = 1

GRID_W = 64
CTX_LEN = 256
N_HEADS = 8
N_KV_HEADS = 2
HEAD_DIM = 64
KV_GROUP = N_HEADS // N_KV_HEADS
ATTN_WIDTH = N_HEADS * HEAD_DIM
KV_WIDTH = N_KV_HEADS * HEAD_DIM
WINDOW = 128
BLOCK = 128
ROPE_THETA = 10000.0
POOL_WINDOWS = (2, 4, 8, 16)
N_POOL_GROUPS = 4
POOL_WIDTH = D_MODEL - ATTN_WIDTH
POOL_GROUP_DIM = POOL_WIDTH // N_POOL_GROUPS
MIX_WIDTH = ATTN_WIDTH + POOL_WIDTH
IN_COLS = ATTN_WIDTH + 2 * KV_WIDTH + POOL_WIDTH
N_EXPERTS = 64
TOP_K = 8
N_EXPERT_GROUPS = 8
EXPERTS_PER_GROUP = N_EXPERTS // N_EXPERT_GROUPS
TOPK_GROUPS = 4
D_EXPERT = 256
D_SHARED = 256
ROUTED_SCALE = 2.5
EXPERT_BLOCK = 128
EPS = 1e-6

kernel_name = "hybrid_window_gqa_pool_moe_dit_layer"

F32 = jnp.float32


def rmsnorm(x, g):
    x32 = x.astype(F32)
    y = x32 * lax.rsqrt(jnp.mean(x32 * x32, axis=-1, keepdims=True) + EPS)
    return (y * g.astype(F32)).astype(x.dtype)


def modulate(h, shift, scale):
    return h * (1 + scale) + shift


def axial_rope_tables(n_tokens):
    rows = n_tokens // GRID_W
    row = jnp.repeat(jnp.arange(rows), GRID_W).astype(F32)
    col = jnp.tile(jnp.arange(GRID_W), rows).astype(F32)
    n_freq = HEAD_DIM // 4
    inv_freq = ROPE_THETA ** (-jnp.arange(n_freq, dtype=F32) / n_freq)
    ang_r = row[:, None] * inv_freq[None, :]
    ang_c = col[:, None] * inv_freq[None, :]
    return jnp.cos(ang_r), jnp.sin(ang_r), jnp.cos(ang_c), jnp.sin(ang_c)


def _rope_half(x, cos, sin):
    x1, x2 = jnp.split(x, 2, axis=-1)
    cos = cos[:, None, :]
    sin = sin[:, None, :]
    return jnp.concatenate([x1 * cos - x2 * sin, x1 * sin + x2 * cos], axis=-1)


def apply_axial_rope(x, tables):
    cos_r, sin_r, cos_c, sin_c = tables
    xr, xc = jnp.split(x.astype(F32), 2, axis=-1)
    return jnp.concatenate([_rope_half(xr, cos_r, sin_r), _rope_half(xc, cos_c, sin_c)], axis=-1).astype(x.dtype)


def split_projection(z):
    B, L = z.shape[:2]
    q = z[..., :ATTN_WIDTH].reshape(B, L, N_HEADS, HEAD_DIM)
    k = z[..., ATTN_WIDTH:ATTN_WIDTH + KV_WIDTH].reshape(B, L, N_KV_HEADS, HEAD_DIM)
    v = z[..., ATTN_WIDTH + KV_WIDTH:ATTN_WIDTH + 2 * KV_WIDTH].reshape(B, L, N_KV_HEADS, HEAD_DIM)
    p = z[..., ATTN_WIDTH + 2 * KV_WIDTH:]
    return q, k, v, p


def split_kv(z):
    B, L = z.shape[:2]
    k = z[..., :KV_WIDTH].reshape(B, L, N_KV_HEADS, HEAD_DIM)
    v = z[..., KV_WIDTH:].reshape(B, L, N_KV_HEADS, HEAD_DIM)
    return k, v


def latent_window_attention(q, k, v, k_ctx, v_ctx, sink):
    B, S = q.shape[:2]
    nb = S // BLOCK
    scale = HEAD_DIM ** -0.5
    qb = q.reshape(B, nb, BLOCK, N_KV_HEADS, KV_GROUP, HEAD_DIM)

    def band(t):
        tp = jnp.pad(t, ((0, 0), (BLOCK, BLOCK), (0, 0), (0, 0))).reshape(B, nb + 2, BLOCK, N_KV_HEADS, HEAD_DIM)
        return jnp.concatenate([tp[:, :-2], tp[:, 1:-1], tp[:, 2:]], axis=2)

    kb, vb = band(k), band(v)
    s_loc = jnp.einsum('bnqkgd,bnjkd->bnkgqj', qb, kb, preferred_element_type=F32) * scale
    s_ctx = jnp.einsum('bnqkgd,bckd->bnkgqc', qb, k_ctx, preferred_element_type=F32) * scale
    q_pos = jnp.arange(nb)[:, None, None] * BLOCK + jnp.arange(BLOCK)[None, :, None]
    k_pos = jnp.arange(nb)[:, None, None] * BLOCK - BLOCK + jnp.arange(3 * BLOCK)[None, None, :]
    allowed = (jnp.abs(q_pos - k_pos) <= WINDOW) & (k_pos >= 0) & (k_pos < S)
    s_loc = jnp.where(allowed[None, :, None, None], s_loc, -jnp.inf)
    sink_l = sink.astype(F32).reshape(1, 1, N_KV_HEADS, KV_GROUP, 1, 1)
    m = jnp.maximum(jnp.maximum(s_loc.max(-1, keepdims=True), s_ctx.max(-1, keepdims=True)), sink_l)
    p_loc = jnp.exp(s_loc - m)
    p_ctx = jnp.exp(s_ctx - m)
    denom = p_loc.sum(-1, keepdims=True) + p_ctx.sum(-1, keepdims=True) + jnp.exp(sink_l - m)
    o = (jnp.einsum('bnkgqj,bnjkd->bnkgqd', p_loc, vb.astype(F32))
         + jnp.einsum('bnkgqc,bckd->bnkgqd', p_ctx, v_ctx.astype(F32))) / denom
    o = o.transpose(0, 1, 4, 2, 3, 5).reshape(B, S, ATTN_WIDTH)
    return o.astype(q.dtype)


def context_attention(q, k, v, sink):
    B, C = q.shape[:2]
    scale = HEAD_DIM ** -0.5
    qc = q.reshape(B, C, N_KV_HEADS, KV_GROUP, HEAD_DIM)
    s = jnp.einsum('bqkgd,bckd->bkgqc', qc, k, preferred_element_type=F32) * scale
    sink_l = sink.astype(F32).reshape(1, N_KV_HEADS, KV_GROUP, 1, 1)
    m = jnp.maximum(s.max(-1, keepdims=True), sink_l)
    p = jnp.exp(s - m)
    denom = p.sum(-1, keepdims=True) + jnp.exp(sink_l - m)
    o = jnp.einsum('bkgqc,bckd->bkgqd', p, v.astype(F32)) / denom
    return o.transpose(0, 3, 1, 2, 4).reshape(B, C, ATTN_WIDTH).astype(q.dtype)


def multiscale_pool(p, pool_w, pool_scale):
    B, L, _ = p.shape
    p32 = p.astype(F32)
    cs = jnp.concatenate([jnp.zeros((B, 1, POOL_WIDTH), F32), jnp.cumsum(p32, axis=1)], axis=1)
    t = jnp.arange(L)
    outs = []
    for g, w in enumerate(POOL_WINDOWS):
        lo = jnp.clip(t - w // 2, 0, L)
        hi = jnp.clip(t - w // 2 + w, 0, L)
        sl = slice(g * POOL_GROUP_DIM, (g + 1) * POOL_GROUP_DIM)
        csg = cs[:, :, sl]
        mean = (csg[:, hi] - csg[:, lo]) / (hi - lo).astype(F32)[None, :, None]
        outs.append(mean - p32[:, :, sl])
    d = jnp.stack(outs, axis=2)
    y = jnp.einsum('blgc,gcd->blgd', d, pool_w.astype(F32)).reshape(B, L, POOL_WIDTH)
    return (y * pool_scale.astype(F32)).astype(p.dtype)


def routed_experts(h, idx, gate, w_gate, w_up, w_down):
    T, D = h.shape
    TK = T * TOP_K
    flat_e = idx.reshape(TK)
    flat_w = gate.reshape(TK)
    order = jnp.argsort(flat_e)
    e_sorted = flat_e[order]
    sizes = jnp.bincount(flat_e, length=N_EXPERTS)
    padded = (sizes + EXPERT_BLOCK - 1) // EXPERT_BLOCK * EXPERT_BLOCK
    pad_end = jnp.cumsum(padded)
    pad_start = pad_end - padded
    start = jnp.cumsum(sizes) - sizes
    dest = pad_start[e_sorted] + (jnp.arange(TK) - start[e_sorted])
    n_rows = -(-(TK + N_EXPERTS * (EXPERT_BLOCK - 1)) // EXPERT_BLOCK) * EXPERT_BLOCK
    n_blocks = n_rows // EXPERT_BLOCK
    row_tok = jnp.zeros((n_rows,), jnp.int32).at[dest].set((order // TOP_K).astype(jnp.int32))
    row_w = jnp.zeros((n_rows,), F32).at[dest].set(flat_w[order])
    block_e = jnp.minimum(jnp.searchsorted(pad_end, jnp.arange(n_blocks) * EXPERT_BLOCK, side='right'),
                          N_EXPERTS - 1)

    def expert_block(args):
        tok_b, w_b, e = args
        xb = h[tok_b]
        y = (jax.nn.silu(xb @ w_gate[e]) * (xb @ w_up[e])) @ w_down[e]
        return y.astype(F32) * w_b[:, None]

    y_rows = lax.map(expert_block, (row_tok.reshape(n_blocks, EXPERT_BLOCK),
                                    row_w.reshape(n_blocks, EXPERT_BLOCK), block_e))
    return jnp.zeros((T, D), F32).at[row_tok].add(y_rows.reshape(n_rows, D))


def moe_ffn(h, w_router, router_bias, w_gate, w_up, w_down, ws_gate, ws_up, ws_down):
    T = h.shape[0]
    scores = jax.nn.sigmoid(jnp.dot(h, w_router, preferred_element_type=F32))
    biased = scores + router_bias.astype(F32)
    grp_score = lax.top_k(biased.reshape(T, N_EXPERT_GROUPS, EXPERTS_PER_GROUP), 2)[0].sum(-1)
    _, top_grp = lax.top_k(grp_score, TOPK_GROUPS)
    grp_mask = jnp.any(top_grp[:, :, None] == jnp.arange(N_EXPERT_GROUPS)[None, None, :], axis=1)
    expert_mask = jnp.repeat(grp_mask, EXPERTS_PER_GROUP, axis=1)
    _, idx = lax.top_k(jnp.where(expert_mask, biased, -jnp.inf), TOP_K)
    gate = jnp.take_along_axis(scores, idx, axis=1)
    gate = gate / jnp.sum(gate, axis=-1, keepdims=True) * ROUTED_SCALE
    routed = routed_experts(h, idx, gate, w_gate, w_up, w_down)
    shared = (jax.nn.silu(h @ ws_gate) * (h @ ws_up)) @ ws_down
    return (routed + shared.astype(F32)).astype(h.dtype)


def setup_inputs(seed: int = 0) -> dict:
    key = jax.random.key(seed)
    ks = jax.random.split(key, 24)
    D, L = D_MODEL, DEPTH

    def nrm(k, shape, scale):
        return jax.random.normal(k, shape, F32) * scale

    return {
        "x": nrm(ks[0], (BATCH, SEQ, D), 1.0),
        "c": nrm(ks[1], (BATCH, D), 1.0),
        "ctx": nrm(ks[2], (BATCH, CTX_LEN, D), 1.0),
        "c_ctx": nrm(ks[3], (D,), 1.0),
        "w_ada": nrm(ks[4], (L, D, 6 * D), 0.5 * D ** -0.5),
        "b_ada": nrm(ks[5], (L, 6 * D), 0.02),
        "norm1_g": 1.0 + nrm(ks[6], (L, D), 0.05),
        "norm2_g": 1.0 + nrm(ks[7], (L, D), 0.05),
        "w_in": nrm(ks[8], (L, D, IN_COLS), D ** -0.5),
        "attn_sink": nrm(ks[9], (L, N_HEADS), 0.5),
        "pool_w": nrm(ks[10], (L, N_POOL_GROUPS, POOL_GROUP_DIM, POOL_GROUP_DIM), POOL_GROUP_DIM ** -0.5),
        "pool_scale": 1.0 + nrm(ks[11], (L, POOL_WIDTH), 0.1),
        "w_out": nrm(ks[12], (L, MIX_WIDTH, D), MIX_WIDTH ** -0.5),
        "w_router": nrm(ks[13], (L, D, N_EXPERTS), D ** -0.5),
        "router_bias": nrm(ks[14], (L, N_EXPERTS), 0.01),
        "w_gate": nrm(ks[15], (L, N_EXPERTS, D, D_EXPERT), D ** -0.5),
        "w_up": nrm(ks[16], (L, N_EXPERTS, D, D_EXPERT), D ** -0.5),
        "w_down": nrm(ks[17], (L, N_EXPERTS, D_EXPERT, D), D_EXPERT ** -0.5),
        "ws_gate": nrm(ks[18], (L, D, D_SHARED), D ** -0.5),
        "ws_up": nrm(ks[19], (L, D, D_SHARED), D ** -0.5),
        "ws_down": nrm(ks[20], (L, D_SHARED, D), D_SHARED ** -0.5),
        "final_g": 1.0 + nrm(ks[21], (D,), 0.05),
    }


def reference(x, c, ctx, c_ctx, w_ada, b_ada, norm1_g, norm2_g, w_in, attn_sink, pool_w, pool_scale,
              w_out, w_router, router_bias, w_gate, w_up, w_down, ws_gate, ws_up, ws_down, final_g):
    B, S, D = x.shape
    rope = axial_rope_tables(S)
    silu_c = jax.nn.silu(c)
    silu_cc = jax.nn.silu(c_ctx)
    for l in range(DEPTH):
        last = l == DEPTH - 1
        mod = silu_c @ w_ada[l] + b_ada[l]
        mod_c = silu_cc @ w_ada[l] + b_ada[l]
        sh1, sc1, g1, sh2, sc2, g2 = jnp.split(mod[:, None, :], 6, axis=-1)
        sh1c, sc1c, g1c, sh2c, sc2c, g2c = jnp.split(mod_c, 6)

        hx = modulate(rmsnorm(x, norm1_g[l]), sh1, sc1)
        hc = modulate(rmsnorm(ctx, norm1_g[l]), sh1c, sc1c)
        qx, kx, vx, px = split_projection(hx @ w_in[l])
        qx = apply_axial_rope(qx, rope)
        kx = apply_axial_rope(kx, rope)
        if last:
            kc, vc = split_kv(hc @ w_in[l][:, ATTN_WIDTH:ATTN_WIDTH + 2 * KV_WIDTH])
        else:
            qc, kc, vc, pc = split_projection(hc @ w_in[l])
        attn_x = latent_window_attention(qx, kx, vx, kc, vc, attn_sink[l])
        pool_x = multiscale_pool(px, pool_w[l], pool_scale[l])
        x = x + g1 * (jnp.concatenate([attn_x, pool_x], axis=-1) @ w_out[l])
        if not last:
            attn_c = context_attention(qc, kc, vc, attn_sink[l])
            pool_c = multiscale_pool(pc, pool_w[l], pool_scale[l])
            ctx = ctx + g1c * (jnp.concatenate([attn_c, pool_c], axis=-1) @ w_out[l])

        hx2 = modulate(rmsnorm(x, norm2_g[l]), sh2, sc2)
        ffn_x = moe_ffn(hx2.reshape(B * S, D), w_router[l], router_bias[l], w_gate[l], w_up[l], w_down[l],
                        ws_gate[l], ws_up[l], ws_down[l]).reshape(B, S, D)
        x = x + g2 * ffn_x
        if not last:
            Cn = ctx.shape[1]
            hc2 = modulate(rmsnorm(ctx, norm2_g[l]), sh2c, sc2c)
            ffn_c = moe_ffn(hc2.reshape(B * Cn, D), w_router[l], router_bias[l], w_gate[l], w_up[l], w_down[l],
                            ws_gate[l], ws_up[l], ws_down[l]).reshape(B, Cn, D)
            ctx = ctx + g2c * ffn_c
    return rmsnorm(x, final_g)
```

```python
import os
import numpy as np
import ml_dtypes
import concourse.bass as bass
import concourse.mybir as mybir
from concourse.bass_utils import run_bass_kernel_spmd

F32, BF16, U8 = mybir.dt.float32, mybir.dt.bfloat16, mybir.dt.uint8
AF = mybir.ActivationFunctionType
ALU = mybir.AluOpType
AX = mybir.AxisListType

NCORES = 8
NU = int(os.environ.get("MK_NU", "4"))
NEXP = int(os.environ.get("MK_NEXP", "64"))
DEBUG = int(os.environ.get("MK_DEBUG", "0"))
STOP = int(os.environ.get("MK_STOP", "9"))
TU, HALO, TT, KC = 1024, 128, 1280, 8
EPS = 1e-6
ENGS = ("pe", "act", "dve", "pool", "sp")


class _Rec:
    def __init__(self):
        self.call = None

    def __getattr__(self, name):
        def f(*a, **k):
            self.call = (name, a, k)
        return f


class Sched:
    def __init__(self, nc):
        self.nc = nc
        self.ops = {e: [] for e in ENGS}
        self.res = {}
        self.dma_count = {}
        self.waited = {e: {} for e in ENGS}
        self.last_compute = {e: -1 for e in ENGS}

    def _res(self, r):
        if r not in self.res:
            self.res[r] = {"w": None, "r": {}}
        return self.res[r]

    def _addwaits(self, eng, deps):
        waits = []
        wd = self.waited[eng]
        for k, i in deps.items():
            if wd.get(k, -1) >= i:
                continue
            wd[k] = i
            waits.append((k, i))
            if k in ENGS:
                self.ops[k][i]["flag"] = True
        return waits

    def op(self, eng, fn, reads=(), writes=(), dma=None):
        if fn is not None:
            rcd = _Rec()
            fn(rcd)
            fn = rcd.call
        lst = self.ops[eng]
        idx = len(lst)
        if dma is not None:
            ck = ("dma", dma)
            cidx = self.dma_count.get(dma, 0) + 1
            self.dma_count[dma] = cidx
        else:
            ck, cidx = eng, idx
        deps = {}

        def add(k, i):
            if deps.get(k, -1) < i:
                deps[k] = i

        for r in reads:
            st = self._res(r)
            if st["w"] is not None:
                add(*st["w"])
        for w in writes:
            st = self._res(w)
            if st["w"] is not None and not (st["w"][0] == ck and eng == "pe" and dma is None):
                add(*st["w"])
            for k, i in st["r"].items():
                if not (k == ck and eng == "pe" and dma is None):
                    add(k, i)
        rec = {"fn": fn, "waits": self._addwaits(eng, deps), "flag": False, "dma": dma}
        lst.append(rec)
        if dma is None and fn is not None:
            self.last_compute[eng] = idx
        for r in reads:
            st = self._res(r)
            if st["r"].get(ck, -1) < cidx:
                st["r"][ck] = cidx
        for w in writes:
            st = self._res(w)
            st["w"] = (ck, cidx)
            st["r"] = {}
        return rec

    def barrier(self):
        last = dict(self.last_compute)
        for e in ENGS:
            deps = {}
            for k in ENGS:
                if k != e and last[k] >= 0:
                    deps[k] = last[k]
            for d, c in self.dma_count.items():
                deps[("dma", d)] = c
            self.ops[e].append({"fn": None, "waits": self._addwaits(e, deps), "flag": False, "dma": None})

    def emit(self):
        nc = self.nc
        for e in ENGS:
            c = 0
            for o in self.ops[e]:
                if o["flag"]:
                    assert o["dma"] is None and o["fn"] is not None
                    c += 1
                    o["val"] = c
        from contextlib import ExitStack
        sems = {}
        with ExitStack() as es:
            for e in ENGS:
                sems[e] = es.enter_context(nc.semaphore("c_" + e))
            for d in self.dma_count:
                sems[("dma", d)] = es.enter_context(nc.semaphore("d_" + d))
            block = es.enter_context(nc.Block())

            def run(e, engobj):
                for o in self.ops[e]:
                    for k, i in o["waits"]:
                        v = self.ops[k][i]["val"] if k in ENGS else 16 * i
                        if v > 0:
                            engobj.wait_ge(sems[k], v)
                    if o["fn"] is None:
                        continue
                    name, a, k = o["fn"]
                    ins = getattr(engobj, name)(*a, **k)
                    if o["dma"] is not None:
                        ins.then_inc(sems[("dma", o["dma"])], 16)
                    elif o["flag"]:
                        ins.then_inc(sems[e], 1)

            @block.tensor
            def _(pe):
                run("pe", pe)

            @block.scalar
            def _(act):
                run("act", act)

            @block.vector
            def _(dve):
                run("dve", dve)

            @block.gpsimd
            def _(pool):
                run("pool", pool)

            @block.sync
            def _(sp):
                run("sp", sp)


def build_program():
    nc = bass.Bass("TRN2", target_bir_lowering=False)
    S = Sched(nc)

    def din(name, shape, dt=F32):
        return nc.dram_tensor(name, list(shape), dt, kind="ExternalInput").ap()

    d_x = din("xT", [NU, 128, KC * TT])
    d_tab = din("tabs", [NU, 128, 2 * TT])
    d_vm = din("vmask", [NU, 128, TT])
    d_mk = din("masks", [NU, 128, 4 * 512], BF16)
    d_invc = din("invc", [NU, 128, 64])
    d_ctx = din("ctxT", [128, KC * 256])
    d_c = din("cT", [128, KC * 2])
    d_wada = din("w_ada", [6, 128, KC * 1024])
    d_bada = din("b_adaT", [128, 48])
    d_gvec = din("gvec", [128, 24])
    d_win = din("w_in", [128, KC * 1920])
    d_wout = din("w_out", [128, KC * 1024])
    d_poolw = din("pool_w", [128, 512])
    d_pscale = din("pool_scale", [128, 4])
    d_wr = din("w_router", [128, KC * 64])
    d_rb = din("rbias", [128, 64])
    d_sink = din("sink", [128, 8])
    if STOP > 4:
        d_wg = din("wg", [65, 128, 2048])
        d_wu = din("wu", [65, 128, 2048])
        d_wd = din("wd", [65, 128, 2048])
    d_ident = din("ident", [128, 128])
    d_out = nc.dram_tensor("outT", [NU, 128, KC * TU], F32, kind="ExternalOutput").ap()
    dbg = {}
    if DEBUG:
        def dout(name, shape, dt=F32):
            dbg[name] = nc.dram_tensor(name, list(shape), dt, kind="ExternalOutput").ap()
        dout("dbg_mod", [128, 96])
        dout("dbg_q", [128, 4 * TT], BF16)
        dout("dbg_k", [128, TT], BF16)
        dout("dbg_v", [128, 10 * 130], BF16)
        dout("dbg_p", [128, 4 * TT])
        dout("dbg_mix", [128, 8 * TU], BF16)
        dout("dbg_x1", [128, KC * TT])
        dout("dbg_h2", [128, 8 * TU], BF16)
        dout("dbg_g", [64, TU])

    ARENA = 208896
    arena = nc.alloc_sbuf_tensor("arena", [128, ARENA], U8)[:]
    cur = [0]

    def alloc(nbytes):
        off = cur[0]
        cur[0] += (nbytes + 63) // 64 * 64
        assert cur[0] <= ARENA, cur[0]
        return off

    def V(off, n, dt, parts=128, p0=0):
        esz = 4 if dt == F32 else 2
        return arena[p0:p0 + parts, off:off + n * esz].bitcast(dt)

    def V3(off, a, b, dt):
        return V(off, a * b, dt).rearrange("p (a b) -> p a b", a=a)

    o_cst = alloc(16 * 8 * 4)
    cst = V3(o_cst, 16, 8, F32)
    o_gvec = alloc(24 * 4); gvec = V3(o_gvec, 3, 8, F32)
    o_mod = alloc(96 * 4); modS = V3(o_mod, 48, 2, F32)
    o_bada = alloc(48 * 4); badaT = V(o_bada, 48, F32)
    o_psc = alloc(16); pscale = V(o_psc, 4, F32)
    o_esink = alloc(32); esink = V(o_esink, 8, F32)
    o_rb = alloc(256); rbias = V(o_rb, 64, F32)
    o_ones = alloc(256); onesb = V(o_ones, 128, BF16)
    o_idf = alloc(512); identf = V(o_idf, 128, F32)
    o_idb = alloc(256); identb = V(o_idb, 128, BF16)
    o_ct = alloc(64); cTt = V3(o_ct, 8, 2, F32)
    o_sct = alloc(64); scT = V3(o_sct, 8, 2, F32)
    o_wr = alloc(KC * 64 * 4); wr = V3(o_wr, 8, 64, F32)
    o_poolw = alloc(512 * 2); poolw = V3(o_poolw, 4, 128, BF16)
    o_kc = alloc(256 * 2); KcT = V(o_kc, 256, BF16)
    o_vc = alloc(2 * 130 * 2); Vc = V(o_vc, 260, BF16).rearrange("p (a g d) -> p a g d", a=2, g=2)
    gscr = nc.dram_tensor("gscr", [64, TU], F32).ap()
    o_wout = alloc(KC * 1024 * 2); wout = V3(o_wout, 8, 1024, BF16)
    o_x = alloc(KC * TT * 4); xT = V3(o_x, 8, TT, F32)
    o_xs = alloc(KC * 512 * 4); xstage = V3(o_xs, 8, 512, F32)
    R1 = alloc(30720)
    R3 = alloc(36864)
    R4a = alloc(24576)
    R4b = alloc(32768)
    win = V3(R1, 8, 1920, BF16)
    pt = [V(R1 + i * 1024, 512, BF16) for i in range(10)]
    attn_tok = [V(R1 + 10240 + i * 1024, 512, BF16) for i in range(2)]
    tA = V(R1 + 12288, TT, F32)
    tB = V(R1 + 12288 + 5120, TT, F32)
    dF = V(R1 + 22528, TU, F32)
    dTb = V(R1 + 26624, TU, BF16)
    denb = V(R1 + 28672, 8, F32)
    recb = V(R1 + 28736, 8, F32)
    tmpE = V(R1 + 28800, 16, F32)
    QT = V3(R3, 4, TT, BF16)
    KT = V(R3 + 10240, TT, BF16)
    Vtok = V(R3 + 12800, 10 * 130, BF16).rearrange("p (a g d) -> p a g d", a=10, g=2)
    pT = V3(R3 + 15424, 4, TT, F32)
    h2T = V3(R3, 8, TU, BF16)
    gT = V(R3 + 16384, TU, F32, parts=64)
    rt = {}
    ro = R3 + 20480
    for nm in ("sc", "bi", "ta", "tb", "msk", "gate"):
        rt[nm] = V(ro, 512, F32); ro += 2048
    for nm in ("m1", "m2", "gs", "t8g", "gm", "t8e"):
        rt[nm] = V(ro, 64, F32); ro += 256
    for nm in ("den", "rden"):
        rt[nm] = V(ro, 8, F32); ro += 64
    assert ro <= R3 + 36864
    Wgu = [V(R4a + s * 12288, 4096, BF16) for s in range(2)]
    Wd = [V(R4a + s * 12288 + 8192, 2048, BF16) for s in range(2)]
    sqA = V3(R4a, 8, 512, BF16)
    hT = [V3(R4a + 8192 + i * 8192, 8, 512, BF16) for i in range(2)]
    tabC = V(R4b, TT, F32)
    tabS = V(R4b + 5120, TT, F32)
    vmask = V(R4b + 10240, TT, F32)
    rstdA = V(R4b + 15360, 512, F32)
    xnA = [V(R4b + 17408 + i * 2048, 512, F32) for i in range(2)]
    rtmp = [V(R4b + 21504 + i * 2048, 512, F32) for i in range(2)]
    mk = V3(R4b + 25600, 4, 512, BF16)
    invc = V3(R4b + 29696, 4, 16, F32)
    mixT = V3(R4b + 4608, 8, TU, BF16)
    sqD = V3(R4b, 8, 512, BF16)
    rstdD = V(R4b + 8192, 512, F32)
    xnD = [V(R4b + 10240 + i * 2048, 512, F32) for i in range(2)]
    h2f = V3(R4b + 14336, 8, 512, F32)
    sgb = [[V(R4b + (p * 2 + f) * 2048, 512, F32) for f in range(2)] for p in range(2)]
    tbuf = [V(R4b + 8192 + i * 2048, 512, F32) for i in range(2)]
    gbs = [V(R4b + 12288 + i * 2048, 512, F32) for i in range(4)]
    ATb = [V3(R4b + 20480 + i * 2048, 2, 512, BF16) for i in range(2)]
    sqF = V3(R4b, 8, 512, BF16)
    rstdF = V(R4b + 8192, 512, F32)
    outT = V3(R4b + 10240, 8, 512, F32)
    wa = [V3(R1 + i * 32768, 8, 1024, F32) for i in range(3)]
    hc = V3(R4a, 8, 256, BF16)
    ctxT = V3(R4b, 8, 256, F32)
    sqC = V3(R4b + 8192, 8, 256, BF16)
    rstdC = V(R4b + 12288, 256, F32)
    xnC = [V(R4b + 13312 + i * 1024, 256, F32) for i in range(2)]

    pb = [nc.alloc_psum_tensor("pb%d" % i, [128, 512], F32)[:] for i in range(7)]
    pbt = nc.alloc_psum_tensor("pbt", [128, 1024], BF16)[:]
    pb7 = pbt.bitcast(F32)

    def dma(eng, out, in_, writes=(), reads=(), sem=None):
        S.op(eng, lambda e: e.dma_start(out=out, in_=in_), reads=reads, writes=writes, dma=sem)

    dma("sp", cTt.rearrange("p a b -> p (a b)"), d_c, ["cT"], sem="ld_c")
    dma("sp", badaT, d_bada, ["bada"], sem="ld_bada")
    dma("sp", gvec.rearrange("p a b -> p (a b)"), d_gvec, ["gvec"], sem="ld_gvec")
    dma("sp", pscale, d_pscale, ["pscale"], sem="ld_psc")
    dma("sp", rbias, d_rb, ["rbias"], sem="ld_rb")
    dma("sp", esink, d_sink, ["esink"], sem="ld_sink")
    dma("sp", identf, d_ident, ["identf"], sem="ld_id")
    dma("pool", identb, d_ident, ["identb"], sem="ld_idb")
    dma("sp", wr.rearrange("p a b -> p (a b)"), d_wr, ["wr"], sem="ld_wr")
    dma("pool", poolw.rearrange("p a b -> p (a b)"), d_poolw, ["poolw"], sem="ld_pw")
    for k in range(8):
        dma("pool", wout[:, k, :], d_wout[:, k * 1024:(k + 1) * 1024], ["wout"], sem="ld_wout")
    S.op("pool", lambda e: e.memset(onesb, 1.0), writes=["onesb"])
    S.op("pool", lambda e: e.memset(Vc[:, :, :, 64:65], 1.0), writes=["Vc1"])
    S.op("act", lambda e: e.activation(out=scT, in_=cTt, func=AF.Silu), reads=["cT"], writes=["scT"])
    S.op("act", lambda e: e.activation(out=esink, in_=esink, func=AF.Exp), reads=["esink"], writes=["esink"])
    psmod = pb[0][:, 0:96].rearrange("p (a b) -> p a b", b=2)
    for v in range(6):
        b = v % 3
        dma("sp", wa[b].rearrange("p a b -> p (a b)"), d_wada[v], ["wa%d" % b], sem="ld_wa%d" % b)
        for m in range(8):
            for k in range(8):
                S.op("pe", lambda e, b=b, m=m, k=k, v=v: e.matmul(
                    psmod[:, v * 8 + m, :], wa[b][:, k, m * 128:(m + 1) * 128], scT[:, k, :],
                    start=(k == 0), stop=(k == 7)), reads=["wa%d" % b, "scT"], writes=["psmod"])
    S.op("dve", lambda e: e.tensor_tensor(out=modS, in0=psmod, in1=badaT.unsqueeze(2).to_broadcast([128, 48, 2]),
                                          op=ALU.add), reads=["psmod", "bada"], writes=["modS"])

    def cst_copy(row, v, col):
        S.op("dve", lambda e: e.tensor_copy(out=cst[:, row, :], in_=modS[:, v * 8:(v + 1) * 8, col]),
             reads=["modS"], writes=["cst"])

    def cst_scale(row, v, col, gi):
        S.op("dve", lambda e: e.scalar_tensor_tensor(out=cst[:, row, :], in0=modS[:, v * 8:(v + 1) * 8, col], scalar=1.0,
                                                     in1=gvec[:, gi, :], op0=ALU.add, op1=ALU.mult),
             reads=["modS", "gvec"], writes=["cst"])

    cst_copy(0, 0, 0); cst_scale(1, 1, 0, 0); cst_copy(2, 2, 0)
    cst_copy(3, 3, 0); cst_scale(4, 4, 0, 1); cst_copy(5, 5, 0)
    cst_copy(6, 0, 1); cst_scale(7, 1, 1, 0)
    S.op("dve", lambda e: e.tensor_copy(out=cst[:, 8, :], in_=gvec[:, 2, :]), reads=["gvec"], writes=["cst"])
    if DEBUG:
        dma("sp", dbg["dbg_mod"], modS.rearrange("p a b -> p (a b)"), reads=["modS"], writes=["o_mod"], sem="st_dbg0")
    S.barrier()

    ss_bank = pb[0]

    def rms_stats(src3, n, sq, rstd, tag, srcn=None):
        srcn = srcn or (tag + "_src")
        S.op("act", lambda e: e.activation(out=sq[:, :, 0:n], in_=src3, func=AF.Square),
             reads=[srcn], writes=[tag + "_sq"])
        for k in range(8):
            S.op("pe", lambda e, k=k: e.matmul(ss_bank[:, 0:n], onesb, sq[:, k, 0:n], start=(k == 0), stop=(k == 7)),
                 reads=[tag + "_sq", "onesb"], writes=["ss_bank"])
        S.op("dve", lambda e: e.tensor_scalar(out=rstd[:, 0:n], in0=ss_bank[:, 0:n], scalar1=1.0 / 1024, scalar2=EPS,
                                              op0=ALU.mult, op1=ALU.add), reads=["ss_bank"], writes=[tag + "_rstd"])
        S.op("act", lambda e: e.activation(out=rstd[:, 0:n], in_=rstd[:, 0:n], func=AF.Sqrt),
             reads=[tag + "_rstd"], writes=[tag + "_rstd"])
        S.op("dve", lambda e: e.reciprocal(out=rstd[:, 0:n], in_=rstd[:, 0:n]), reads=[tag + "_rstd"], writes=[tag + "_rstd"])

    def modulate(src3, n, rstd, xn, arow, brow, dst3, tag, dst_name, srcn=None):
        srcn = srcn or (tag + "_src")
        for k in range(8):
            b = k % 2
            S.op("dve", lambda e, k=k, b=b: e.tensor_tensor(out=xn[b][:, 0:n], in0=src3[:, k, :], in1=rstd[:, 0:n], op=ALU.mult),
                 reads=[srcn, tag + "_rstd"], writes=[tag + "_xn%d" % b])
            S.op("act", lambda e, k=k, b=b: e.activation(out=dst3[:, k, :], in_=xn[b][:, 0:n], func=AF.Identity,
                                                         bias=cst[:, brow, k:k + 1], scale=cst[:, arow, k:k + 1]),
                 reads=[tag + "_xn%d" % b, "cst"], writes=[dst_name])

    def load_win():
        for k in range(8):
            dma("pool", win[:, k, :], d_win[:, k * 1920:(k + 1) * 1920], ["win"], sem="ld_win")

    load_win()
    dma("sp", ctxT.rearrange("p a b -> p (a b)"), d_ctx, ["C_src"], sem="ld_ctx")
    rms_stats(ctxT, 256, sqC, rstdC, "C")
    modulate(ctxT, 256, rstdC, xnC, 7, 6, hc, "C", "hc")
    OC_K, OC_KP, OC_P, OC_V = 8, 9, 10, 14
    for k in range(8):
        S.op("pe", lambda e, k=k: e.matmul(pb[1][:, 0:256], win[:, k, OC_K * 128:(OC_K + 1) * 128], hc[:, k, :],
                                           start=(k == 0), stop=(k == 7)), reads=["win", "hc"], writes=["pb1"])
    S.op("act", lambda e: e.copy(out=KcT, in_=pb[1][:, 0:256]), reads=["pb1"], writes=["KcT"])
    for cb in range(2):
        for k in range(8):
            S.op("pe", lambda e, k=k, cb=cb: e.matmul(pb[2][:, cb * 128:(cb + 1) * 128], hc[:, k, cb * 128:(cb + 1) * 128],
                                                      win[:, k, OC_V * 128:(OC_V + 1) * 128], start=(k == 0), stop=(k == 7)),
                 reads=["win", "hc"], writes=["pb2"])
        S.op("act", lambda e, cb=cb: e.copy(out=Vc[:, cb, :, 0:64],
                                            in_=pb[2][:, cb * 128:(cb + 1) * 128].rearrange("p (g d) -> p g d", g=2)),
             reads=["pb2"], writes=["Vc"])
    S.barrier()

    CH = [(0, 512), (512, 512), (1024, 256)]
    if STOP <= 0:
        NUL = 0
    else:
        NUL = NU

    def w_dma(e_idx, s):
        dma("pool", Wgu[s][:, 0:2048], d_wg[e_idx], ["Wgu%d" % s], sem="ld_wg%d" % s)
        dma("pool", Wgu[s][:, 2048:4096], d_wu[e_idx], ["Wgu%d" % s], sem="ld_wu%d" % s)
        dma("pool", Wd[s], d_wd[e_idx], ["Wd%d" % s], sem="ld_wd%d" % s)

    elist = list(range(NEXP)) + [64]

    for u in range(NUL):
        dx3 = d_x[u].rearrange("p (a b) -> p a b", a=8)
        for ci, (c0, n) in enumerate(CH):
            if ci == 0 and u > 0:
                S.op("pool", lambda e: e.tensor_copy(out=xT[:, :, 0:512], in_=xstage), reads=["xs"], writes=["xT"])
                continue
            dma("sp", xT[:, :, c0:c0 + n], dx3[:, :, c0:c0 + n], ["A%d_src" % ci, "xT"], sem="ld_x%d" % ci)
        dma("sp", mk.rearrange("p a b -> p (a b)"), d_mk[u], ["mk"], sem="ld_mk")
        dma("sp", invc.rearrange("p a b -> p (a b)"), d_invc[u], ["invc"], sem="ld_invc")
        dma("sp", tabC, d_tab[u][:, 0:TT], ["tabC"], sem="ld_tc")
        dma("sp", tabS, d_tab[u][:, TT:2 * TT], ["tabS"], sem="ld_ts")
        dma("sp", vmask, d_vm[u], ["vmask"], sem="ld_vm")
        S.op("pool", lambda e: e.memset(Vtok[:, :, :, 64:65], 1.0), writes=["Vtok1"])
        def normA(ci):
            c0, n = CH[ci]
            hb = hT[ci % 2]
            hn = "hT%d" % (ci % 2)
            src3 = xT[:, :, c0:c0 + n]
            srcn = "A%d_src" % ci
            if ci == 0 and u > 0:
                src3, srcn = xstage, "xs"
            rms_stats(src3, n, sqA, rstdA, "A", srcn)
            modulate(src3, n, rstdA, xnA, 1, 0, hb[:, :, 0:n], "A", hn, srcn)

        def projA(ci):
            c0, n = CH[ci]
            hb = hT[ci % 2]
            hn = "hT%d" % (ci % 2)

            def proj(bank, oc, bname):
                for k in range(8):
                    S.op("pe", lambda e, k=k: e.matmul(pb[bank][:, 0:n], win[:, k, oc * 128:(oc + 1) * 128], hb[:, k, 0:n],
                                                       start=(k == 0), stop=(k == 7)), reads=["win", hn], writes=[bname])

            for pi in range(5):
                ba, bb = (1, 2) if pi % 2 == 0 else (3, 4)
                oc_a, oc_b = (pi, 4 + pi) if pi < 4 else (OC_K, OC_KP)
                proj(ba, oc_a, "pb%d" % ba)
                proj(bb, oc_b, "pb%d" % bb)
                dst = QT[:, pi, c0:c0 + n] if pi < 4 else KT[:, c0:c0 + n]
                dname = "QT" if pi < 4 else "KT"
                S.op("dve", lambda e, ba=ba: e.tensor_tensor(out=rtmp[0][:, 0:n], in0=pb[ba][:, 0:n], in1=tabC[:, c0:c0 + n], op=ALU.mult),
                     reads=["pb%d" % ba, "tabC"], writes=["rtmp0"])
                S.op("dve", lambda e, bb=bb: e.tensor_tensor(out=rtmp[1][:, 0:n], in0=pb[bb][:, 0:n], in1=tabS[:, c0:c0 + n], op=ALU.mult),
                     reads=["pb%d" % bb, "tabS"], writes=["rtmp1"])
                S.op("dve", lambda e, dst=dst: e.tensor_tensor(out=dst, in0=rtmp[0][:, 0:n], in1=rtmp[1][:, 0:n], op=ALU.add),
                     reads=["rtmp0", "rtmp1"], writes=[dname])
            for g in range(4):
                bk = 5
                proj(bk, OC_P + g, "pb%d" % bk)
                S.op("dve", lambda e, g=g, bk=bk: e.tensor_tensor(out=pT[:, g, c0:c0 + n], in0=pb[bk][:, 0:n], in1=vmask[:, c0:c0 + n], op=ALU.mult),
                     reads=["pb%d" % bk, "vmask"], writes=["pT"])
            for j in range(n // 128):
                blk = c0 // 128 + j
                reg = pb[6][:, (j % 2) * 128:(j % 2) * 128 + 128]
                rn = "pb6"
                for k in range(8):
                    S.op("pe", lambda e, k=k, j=j, reg=reg: e.matmul(reg, hb[:, k, j * 128:(j + 1) * 128], win[:, k, OC_V * 128:(OC_V + 1) * 128],
                                                                     start=(k == 0), stop=(k == 7)), reads=["win", hn], writes=[rn])
                S.op("act", lambda e, blk=blk, reg=reg: e.copy(out=Vtok[:, blk, :, 0:64], in_=reg.rearrange("p (g d) -> p g d", g=2)),
                     reads=[rn], writes=["Vtok"])

        normA(0)
        normA(1)
        projA(0)
        normA(2)
        projA(1)
        projA(2)
        if DEBUG and u == 0:
            S.barrier()
            dma("sp", dbg["dbg_q"], QT.rearrange("p a b -> p (a b)"), reads=["QT"], writes=["o1"], sem="st_dbg1")
            dma("sp", dbg["dbg_k"], KT, reads=["KT"], writes=["o2"], sem="st_dbg2")
            dma("sp", dbg["dbg_v"], Vtok.rearrange("p a g d -> p (a g d)"), reads=["Vtok"], writes=["o3"], sem="st_dbg3")
            dma("sp", dbg["dbg_p"], pT.rearrange("p a b -> p (a b)"), reads=["pT"], writes=["o4"], sem="st_dbg4")
        S.barrier()
        if STOP <= 1:
            break
        for i in range(min(2, len(elist)) if STOP > 4 else 0):
            w_dma(elist[i], i)
        def pool_group(gi):
            w = 2 ** (gi + 1)
            p = pT[:, gi, :]
            S.op("pool", lambda e, p=p: e.tensor_tensor(out=tA[:, 1:TT], in0=p[:, 0:TT - 1], in1=p[:, 1:TT], op=ALU.add),
                 reads=["pT"], writes=["tA"])
            Sb, sname = tA, "tA"
            if gi >= 1:
                S.op("pool", lambda e: e.tensor_tensor(out=tB[:, 2:1279], in0=tA[:, 1:1278], in1=tA[:, 3:1280], op=ALU.add),
                     reads=["tA"], writes=["tB"])
                Sb, sname = tB, "tB"
            if gi >= 2:
                S.op("pool", lambda e: e.tensor_tensor(out=tA[:, 4:1277], in0=tB[:, 2:1275], in1=tB[:, 6:1279], op=ALU.add),
                     reads=["tB"], writes=["tA"])
                Sb, sname = tA, "tA"
            if gi >= 3:
                S.op("pool", lambda e: e.tensor_tensor(out=tB[:, 8:1273], in0=tA[:, 4:1269], in1=tA[:, 12:1277], op=ALU.add),
                     reads=["tA"], writes=["tB"])
                Sb, sname = tB, "tB"
            S.op("dve", lambda e, Sb=Sb, p=p, w=w: e.scalar_tensor_tensor(out=dF, in0=Sb[:, 128:1152], scalar=1.0 / w, in1=p[:, 128:1152],
                                                                          op0=ALU.mult, op1=ALU.subtract), reads=[sname, "pT"], writes=["dF"])
            for (eo, so) in ((0, 0), (1016, 8)):
                S.op("dve", lambda e, Sb=Sb, eo=eo, so=so, gi=gi: e.tensor_tensor(out=tmpE[:, so:so + 8], in0=Sb[:, 128 + eo:136 + eo],
                                                                                  in1=invc[:, gi, so:so + 8], op=ALU.mult),
                     reads=[sname, "invc"], writes=["tmpE%d" % so])
                S.op("dve", lambda e, p=p, eo=eo, so=so: e.tensor_tensor(out=dF[:, eo:eo + 8], in0=tmpE[:, so:so + 8], in1=p[:, 128 + eo:136 + eo],
                                                                         op=ALU.subtract), reads=["tmpE%d" % so, "pT", "dF"], writes=["dF"])
            S.op("act", lambda e: e.copy(out=dTb, in_=dF), reads=["dF"], writes=["dTb"])
            for tc in range(2):
                bank = 1 + (gi * 2 + tc) % 3
                S.op("pe", lambda e, bank=bank, gi=gi, tc=tc: e.matmul(pb[bank], poolw[:, gi, :], dTb[:, tc * 512:(tc + 1) * 512], start=True, stop=True),
                     reads=["poolw", "dTb"], writes=["pb%d" % bank])
                S.op("act", lambda e, bank=bank, gi=gi, tc=tc: e.activation(out=mixT[:, 4 + gi, tc * 512:(tc + 1) * 512], in_=pb[bank], func=AF.Identity,
                                                                            scale=pscale[:, gi:gi + 1]), reads=["pb%d" % bank, "pscale"], writes=["mixT"])
        sbank = [0, 1, 2, 3, 6]
        sb_i = [0]
        stages = [(i, g) for i in range(8) for g in range(2)]

        def kbs_of(i):
            return [("l", i), ("l", i + 1), ("l", i + 2), ("c", 0), ("c", 1)]

        def qk_stage(i, g):
            qc0 = 128 + 128 * i
            pr0 = 64 * g
            for kbi, (kind, kb) in enumerate(kbs_of(i)):
                bank = sbank[sb_i[0] % 5]
                sb_i[0] += 1
                lhsT = KT[pr0:pr0 + 64, kb * 128:(kb + 1) * 128] if kind == "l" else KcT[pr0:pr0 + 64, kb * 128:(kb + 1) * 128]
                rhs = QT[pr0:pr0 + 64, :, qc0:qc0 + 128]
                slot = pt[g * 5 + kbi]
                sn = "pt%d" % (g * 5 + kbi)
                S.op("pe", lambda e: e.matmul(pb[bank], lhsT, rhs, start=True, stop=True),
                     reads=["KT", "KcT", "QT"], writes=["pb%d" % bank])
                S.op("act", lambda e: e.activation(out=slot, in_=pb[bank], func=AF.Exp, scale=0.125),
                     reads=["pb%d" % bank], writes=[sn])
                mi = None
                if kbi == 0:
                    mi = 2 if i == 0 else 0
                elif kbi == 2:
                    mi = 3 if i == 7 else 1
                if mi is not None:
                    S.op("dve", lambda e: e.tensor_tensor(out=slot, in0=slot, in1=mk[:, mi, :], op=ALU.mult),
                         reads=[sn, "mk"], writes=[sn])

        def pv_stage(i, g):
            at = attn_tok[i % 2]
            atn = "attn%d" % (i % 2)
            ob = 4 + g
            pso = pb[ob][:, 0:260].rearrange("p (h d) -> p h d", h=4)
            kbs = kbs_of(i)
            for hh in range(4):
                for kbi, (kind, kb) in enumerate(kbs):
                    vx = Vtok[:, kb, g, :] if kind == "l" else Vc[:, kb, g, :]
                    slot = pt[g * 5 + kbi]
                    S.op("pe", lambda e: e.matmul(pso[:, hh, :], slot[:, hh * 128:(hh + 1) * 128], vx, start=(kbi == 0), stop=(kbi == 4)),
                         reads=["pt%d" % (g * 5 + kbi), "Vtok", "Vtok1", "Vc", "Vc1"], writes=["pb%d" % ob])
            S.op("dve", lambda e: e.tensor_tensor(out=denb[:, 4 * g:4 * g + 4], in0=pso[:, :, 64], in1=esink[:, 4 * g:4 * g + 4], op=ALU.add),
                 reads=["pb%d" % ob, "esink"], writes=["den%d" % g])
            S.op("dve", lambda e: e.reciprocal(out=recb[:, 4 * g:4 * g + 4], in_=denb[:, 4 * g:4 * g + 4]),
                 reads=["den%d" % g], writes=["rec%d" % g])
            for hh in range(4):
                h = 4 * g + hh
                S.op("dve", lambda e: e.tensor_scalar(out=at[:, h * 64:(h + 1) * 64], in0=pso[:, hh, 0:64], scalar1=recb[:, h:h + 1], scalar2=None, op0=ALU.mult),
                     reads=["pb%d" % ob, "rec%d" % g], writes=[atn])

        def finish_block(i):
            at = attn_tok[i % 2]
            atn = "attn%d" % (i % 2)
            for c2 in range(4):
                S.op("pe", lambda e: e.transpose(pbt[:, c2 * 128:(c2 + 1) * 128], at[:, c2 * 128:(c2 + 1) * 128], identb),
                     reads=[atn, "identb"], writes=["pbt"])
            S.op("act", lambda e: e.copy(out=mixT[:, 0:4, i * 128:(i + 1) * 128], in_=pbt[:, 0:512].rearrange("p (c q) -> p c q", c=4)),
                 reads=["pbt"], writes=["mixT"])
            if i < 4:
                pool_group(i)

        qk_stage(*stages[0])
        pending = None
        for si_, (i, g) in enumerate(stages):
            if si_ + 1 < len(stages):
                qk_stage(*stages[si_ + 1])
            pv_stage(i, g)
            if pending is not None:
                finish_block(pending)
                pending = None
            if g == 1:
                pending = i
        if pending is not None:
            finish_block(pending)
        if DEBUG and u == 0:
            S.barrier()
            dma("sp", dbg["dbg_mix"], mixT.rearrange("p a b -> p (a b)"), reads=["mixT"], writes=["o5"], sem="st_dbg5")
        S.barrier()
        if STOP <= 2:
            break
        if u + 1 < NU:
            load_win()
        bi_ = 0
        for tc in range(2):
            for m in range(8):
                bank = 1 + bi_ % 3
                bi_ += 1
                for f in range(8):
                    S.op("pe", lambda e, bank=bank, m=m, f=f, tc=tc: e.matmul(pb[bank], wout[:, f, m * 128:(m + 1) * 128], mixT[:, f, tc * 512:(tc + 1) * 512],
                                                                              start=(f == 0), stop=(f == 7)), reads=["wout", "mixT"], writes=["pb%d" % bank])
                xs = xT[:, m, 128 + tc * 512:128 + (tc + 1) * 512]
                S.op("dve", lambda e, bank=bank, m=m, xs=xs: e.scalar_tensor_tensor(out=xs, in0=pb[bank], scalar=cst[:, 2, m:m + 1], in1=xs,
                                                                                    op0=ALU.mult, op1=ALU.add),
                     reads=["pb%d" % bank, "cst", "xT"], writes=["xT", "D_src"])
        if DEBUG and u == 0:
            S.barrier()
            dma("sp", dbg["dbg_x1"], xT.rearrange("p a b -> p (a b)"), reads=["xT"], writes=["o6"], sem="st_dbg6")
        S.barrier()
        if STOP <= 3:
            break
        psr8 = pb[4]
        for tc in range(2):
            src3 = xT[:, :, 128 + tc * 512:128 + (tc + 1) * 512]
            rms_stats(src3, 512, sqD, rstdD, "D")
            modulate(src3, 512, rstdD, xnD, 4, 3, h2f, "D", "h2f")
            S.op("act", lambda e: e.copy(out=h2T[:, :, tc * 512:(tc + 1) * 512], in_=h2f), reads=["h2f"], writes=["h2T"])
            for j in range(4):
                blk = tc * 4 + j
                for k in range(8):
                    S.op("pe", lambda e: e.matmul(psr8[:, blk * 64:(blk + 1) * 64], h2f[:, k, j * 128:(j + 1) * 128], wr[:, k, :],
                                                  start=(k == 0), stop=(k == 7)), reads=["h2f", "wr"], writes=["pb4"])
        R = rt
        v8 = lambda a: a.rearrange("p (j e) -> p j e", j=8)
        v64 = lambda a: a.rearrange("p (g i) -> p g i", i=8)
        g8 = lambda a: a.rearrange("p (j g) -> p j g", j=8)
        S.op("act", lambda e: e.activation(out=R["sc"], in_=psr8, func=AF.Sigmoid), reads=["pb4"], writes=["r_sc"])
        S.op("dve", lambda e: e.tensor_tensor(out=v8(R["bi"]), in0=v8(R["sc"]), in1=rbias.unsqueeze(1).to_broadcast([128, 8, 64]), op=ALU.add),
             reads=["r_sc", "rbias"], writes=["r_bi"])
        S.op("dve", lambda e: e.tensor_reduce(out=R["m1"], in_=v64(R["bi"]), axis=AX.X, op=ALU.max), reads=["r_bi"], writes=["r_m1"])
        S.op("dve", lambda e: e.tensor_tensor(out=v64(R["ta"]), in0=v64(R["bi"]), in1=R["m1"].unsqueeze(2).to_broadcast([128, 64, 8]), op=ALU.is_equal),
             reads=["r_bi", "r_m1"], writes=["r_ta"])
        S.op("dve", lambda e: e.scalar_tensor_tensor(out=R["tb"], in0=R["ta"], scalar=-1e9, in1=R["bi"], op0=ALU.mult, op1=ALU.add),
             reads=["r_ta", "r_bi"], writes=["r_tb"])
        S.op("dve", lambda e: e.tensor_reduce(out=R["m2"], in_=v64(R["tb"]), axis=AX.X, op=ALU.max), reads=["r_tb"], writes=["r_m2"])
        S.op("dve", lambda e: e.tensor_tensor(out=R["gs"], in0=R["m1"], in1=R["m2"], op=ALU.add), reads=["r_m1", "r_m2"], writes=["r_gs"])
        for j in range(8):
            S.op("dve", lambda e: e.max(out=R["t8g"][:, j * 8:(j + 1) * 8], in_=R["gs"][:, j * 8:(j + 1) * 8]), reads=["r_gs"], writes=["r_t8g"])
        S.op("dve", lambda e: e.tensor_tensor(out=g8(R["gm"]), in0=g8(R["gs"]), in1=g8(R["t8g"])[:, :, 3:4].to_broadcast([128, 8, 8]), op=ALU.is_ge),
             reads=["r_gs", "r_t8g"], writes=["r_gm"])
        S.op("dve", lambda e: e.scalar_tensor_tensor(out=v64(R["msk"]), in0=v64(R["bi"]), scalar=2.0, in1=R["gm"].unsqueeze(2).to_broadcast([128, 64, 8]),
                                                     op0=ALU.add, op1=ALU.mult), reads=["r_bi", "r_gm"], writes=["r_msk"])
        for j in range(8):
            S.op("dve", lambda e: e.max(out=R["t8e"][:, j * 8:(j + 1) * 8], in_=R["msk"][:, j * 64:(j + 1) * 64]), reads=["r_msk"], writes=["r_t8e"])
        S.op("dve", lambda e: e.tensor_tensor(out=v8(R["ta"]), in0=v8(R["msk"]), in1=g8(R["t8e"])[:, :, 7:8].to_broadcast([128, 8, 64]), op=ALU.is_ge),
             reads=["r_msk", "r_t8e"], writes=["r_ta"])
        S.op("dve", lambda e: e.tensor_tensor(out=R["tb"], in0=R["ta"], in1=R["sc"], op=ALU.mult), reads=["r_ta", "r_sc"], writes=["r_tb"])
        S.op("dve", lambda e: e.tensor_reduce(out=R["den"], in_=v8(R["tb"]), axis=AX.X, op=ALU.add), reads=["r_tb"], writes=["r_den"])
        S.op("dve", lambda e: e.reciprocal(out=R["rden"], in_=R["den"]), reads=["r_den"], writes=["r_rden"])
        S.op("dve", lambda e: e.scalar_tensor_tensor(out=v8(R["gate"]), in0=v8(R["tb"]), scalar=2.5, in1=R["rden"].unsqueeze(2).to_broadcast([128, 8, 64]),
                                                     op0=ALU.mult, op1=ALU.mult), reads=["r_tb", "r_rden"], writes=["r_gate"])
        for hb_ in range(2):
            tb_ = pb[6] if hb_ == 0 else pb[5]
            tn_ = "pb6" if hb_ == 0 else "pb5"
            for j in range(4):
                blk = hb_ * 4 + j
                S.op("pe", lambda e: e.transpose(tb_[0:64, j * 128:(j + 1) * 128], R["gate"][:, blk * 64:(blk + 1) * 64], identf),
                     reads=["r_gate", "identf"], writes=[tn_])
            S.op("act", lambda e: e.copy(out=gT[:, hb_ * 512:(hb_ + 1) * 512], in_=tb_[0:64, :]), reads=[tn_], writes=["gT"])
        dma("sp", gscr, gT, reads=["gT"], writes=["gscr"], sem="st_gs")
        if DEBUG and u == 0:
            S.barrier()
            dma("sp", dbg["dbg_h2"], h2T.rearrange("p a b -> p (a b)"), reads=["h2T"], writes=["o7"], sem="st_dbg7")
            dma("sp", dbg["dbg_g"], gT, reads=["gT"], writes=["o8"], sem="st_dbg8")
        S.barrier()
        if STOP <= 4:
            break
        steps = [(ei, tc) for ei in range(len(elist)) for tc in range(2)]
        ybank = [0, 6]
        yb_i = [0]

        def rec_gu(si, f, which):
            ei, tc = steps[si]
            s = ei % 2
            bank = 1 + f * 2 + which
            woff = which * 2048
            for k in range(8):
                S.op("pe", lambda e, k=k: e.matmul(pb[bank], Wgu[s][:, woff + k * 256 + f * 128: woff + k * 256 + (f + 1) * 128],
                                                   h2T[:, k, tc * 512:(tc + 1) * 512], start=(k == 0), stop=(k == 7)),
                     reads=["Wgu%d" % s, "h2T"], writes=["pb%d" % bank])

        def rec_evac(si, f):
            ei, tc = steps[si]
            par = si % 2
            e_id = elist[ei]
            gbank, ubank = 1 + f * 2, 2 + f * 2
            sg = sgb[par][f]
            sgn = "sg%d%d" % (par, f)
            S.op("act", lambda e: e.activation(out=sg, in_=pb[gbank], func=AF.Silu), reads=["pb%d" % gbank], writes=[sgn])
            at = ATb[par][:, f, :]
            atn = "AT%d" % par
            if e_id == 64:
                S.op("dve", lambda e: e.tensor_tensor(out=at, in0=pb[ubank], in1=sg, op=ALU.mult), reads=["pb%d" % ubank, sgn], writes=[atn])
            else:
                tb = tbuf[f]
                S.op("dve", lambda e: e.tensor_tensor(out=tb, in0=pb[ubank], in1=gbs[si % 4], op=ALU.mult),
                     reads=["pb%d" % ubank, "gbs%d" % (si % 4)], writes=["tb%d" % f])
                S.op("dve", lambda e: e.tensor_tensor(out=at, in0=tb, in1=sg, op=ALU.mult), reads=["tb%d" % f, sgn], writes=[atn])

        def rec_gb(si):
            ei, tc = steps[si]
            e_id = elist[ei]
            if e_id == 64:
                return
            sl = si % 4
            dma("sp", gbs[sl], gscr[e_id:e_id + 1, tc * 512:(tc + 1) * 512].partition_broadcast(128),
                reads=["gscr"], writes=["gbs%d" % sl], sem="ld_gb%d" % sl)

        def rec_d(si, m):
            ei, tc = steps[si]
            s = ei % 2
            par = si % 2
            bank = ybank[yb_i[0] % 2]
            yb_i[0] += 1
            for f in range(2):
                S.op("pe", lambda e, f=f: e.matmul(pb[bank], Wd[s][:, f * 1024 + m * 128: f * 1024 + (m + 1) * 128], ATb[par][:, f, :],
                                                   start=(f == 0), stop=(f == 1)), reads=["Wd%d" % s, "AT%d" % par], writes=["pb%d" % bank])
            xs = xT[:, m, 128 + tc * 512:128 + (tc + 1) * 512]
            S.op("dve", lambda e: e.scalar_tensor_tensor(out=xs, in0=pb[bank], scalar=cst[:, 5, m:m + 1], in1=xs, op0=ALU.mult, op1=ALU.add),
                 reads=["pb%d" % bank, "cst", "xT"], writes=["xT"])

        nst = len(steps)
        if u + 1 < NU:
            dma("sp", xstage, d_x[u + 1].rearrange("p (a b) -> p a b", a=8)[:, :, 0:512], ["xs"], sem="ld_xs")
        for si in range(min(3, nst)):
            rec_gb(si)
        for si in range(nst + 1):
            if si + 3 < nst:
                rec_gb(si + 3)
            for f in range(2):
                for which in range(2):
                    if si < nst:
                        rec_gu(si, f, which)
                        if which == 1:
                            rec_evac(si, f)
                    if si >= 1:
                        mbase = (f * 2 + which) * 2
                        rec_d(si - 1, mbase)
                        rec_d(si - 1, mbase + 1)
            if si < nst:
                ei, tc = steps[si]
                if tc == 0 and ei >= 1 and ei + 1 < len(elist):
                    w_dma(elist[ei + 1], (ei + 1) % 2)
        S.barrier()
        for tc in range(2):
            src3 = xT[:, :, 128 + tc * 512:128 + (tc + 1) * 512]
            S.op("act", lambda e: e.activation(out=sqF, in_=src3, func=AF.Square),
                 reads=["xT"], writes=["F_sq"])
            for k in range(8):
                S.op("pe", lambda e, k=k: e.matmul(ss_bank, onesb, sqF[:, k, :], start=(k == 0), stop=(k == 7)),
                     reads=["F_sq", "onesb"], writes=["ss_bank"])
            S.op("dve", lambda e: e.tensor_scalar(out=rstdF, in0=ss_bank, scalar1=1.0 / 1024, scalar2=EPS, op0=ALU.mult, op1=ALU.add),
                 reads=["ss_bank"], writes=["F_rstd"])
            S.op("act", lambda e: e.activation(out=rstdF, in_=rstdF, func=AF.Sqrt), reads=["F_rstd"], writes=["F_rstd"])
            S.op("dve", lambda e: e.reciprocal(out=rstdF, in_=rstdF), reads=["F_rstd"], writes=["F_rstd"])
            for k in range(8):
                S.op("dve", lambda e, k=k: e.scalar_tensor_tensor(out=outT[:, k, :], in0=src3[:, k, :], scalar=cst[:, 8, k:k + 1], in1=rstdF,
                                                                  op0=ALU.mult, op1=ALU.mult), reads=["xT", "cst", "F_rstd"], writes=["outT"])
            dma("sp", d_out[u].rearrange("p (a b) -> p a b", a=8)[:, :, tc * 512:(tc + 1) * 512], outT, reads=["outT"], writes=["o_out"], sem="st_out")
        S.barrier()

    S.emit()
    return nc


def _partner():
    pi = np.zeros(64, np.int64)
    for d in range(64):
        dd = d % 32
        pi[d] = d + 16 if dd < 16 else d - 16
    return pi


def _prep_shared(inp):
    f32 = np.float32
    sh = {}
    sh["w_ada"] = np.ascontiguousarray(inp["w_ada"][0].reshape(8, 128, 6, 1024).transpose(2, 1, 0, 3)).reshape(6, 128, 8192)
    sh["b_adaT"] = np.ascontiguousarray(inp["b_ada"][0].reshape(48, 128).T)
    gv = np.stack([inp["norm1_g"][0], inp["norm2_g"][0], inp["final_g"]], 0)
    sh["gvec"] = np.ascontiguousarray(gv.reshape(3, 8, 128).transpose(2, 0, 1)).reshape(128, 24)
    pi = _partner()
    cols = []
    for c in range(4):
        cols += [c * 64 + d for d in range(64)] + [(4 + c) * 64 + d for d in range(64)]
    for c in range(4):
        cols += [c * 64 + pi[d] for d in range(64)] + [(4 + c) * 64 + pi[d] for d in range(64)]
    cols += [512 + i for i in range(128)]
    cols += [512 + h * 64 + pi[d] for h in range(2) for d in range(64)]
    cols += [768 + i for i in range(512)]
    cols += [640 + i for i in range(128)]
    wl = inp["w_in"][0][:, np.array(cols)]
    sh["w_in"] = np.ascontiguousarray(wl.reshape(8, 128, 1920).transpose(1, 0, 2)).reshape(128, 8 * 1920)
    sh["w_out"] = np.ascontiguousarray(inp["w_out"][0].reshape(8, 128, 1024).transpose(1, 0, 2)).reshape(128, 8192)
    sh["pool_w"] = np.ascontiguousarray(inp["pool_w"][0].transpose(1, 0, 2)).reshape(128, 512)
    sh["pool_scale"] = np.ascontiguousarray(inp["pool_scale"][0].reshape(4, 128).T)
    sh["w_router"] = np.ascontiguousarray(inp["w_router"][0].reshape(8, 128, 64).transpose(1, 0, 2)).reshape(128, 512)
    sh["rbias"] = np.ascontiguousarray(np.broadcast_to(inp["router_bias"][0][None, :], (128, 64))).astype(f32)
    sh["sink"] = np.ascontiguousarray(np.broadcast_to(inp["attn_sink"][0][None, :], (128, 8))).astype(f32)
    sh["ident"] = np.eye(128, dtype=f32)
    if STOP <= 4:
        return sh
    wg = np.concatenate([inp["w_gate"][0], inp["ws_gate"]], 0)
    wu = np.concatenate([inp["w_up"][0], inp["ws_up"]], 0)
    wd = np.concatenate([inp["w_down"][0], inp["ws_down"]], 0)
    sh["wg"] = np.ascontiguousarray(wg.reshape(65, 8, 128, 256).transpose(0, 2, 1, 3)).reshape(65, 128, 2048)
    sh["wu"] = np.ascontiguousarray(wu.reshape(65, 8, 128, 256).transpose(0, 2, 1, 3)).reshape(65, 128, 2048)
    sh["wd"] = np.ascontiguousarray(wd.reshape(65, 2, 128, 1024).transpose(0, 2, 1, 3)).reshape(65, 128, 2048)
    return sh


def _prep_core(inp, c):
    f32 = np.float32
    b, half = c // 2, c % 2
    L = 8192
    x = inp["x"][b]
    xpad = np.zeros((L + 256, 1024), f32)
    xpad[128:128 + L] = x
    inv_freq = (np.float32(10000.0) ** (-np.arange(16, dtype=f32) / np.float32(16))).astype(f32)
    pi = _partner()
    xs, tabs, vms, mks, invcs = [], [], [], [], []
    kk = np.arange(128)[:, None]
    qq = np.arange(128)[None, :]
    tri_prev = np.tile((qq <= kk).astype(f32), (1, 4))
    tri_next = np.tile((kk <= qq).astype(f32), (1, 4))
    for u in range(NU):
        base = half * 4096 + u * TU
        seg = xpad[base:base + TT]
        xs.append(np.ascontiguousarray(seg.T.reshape(8, 128, TT).transpose(1, 0, 2)).reshape(128, 8 * TT))
        pos = base - 128 + np.arange(TT)
        valid = ((pos >= 0) & (pos < L))
        posc = np.clip(pos, 0, L - 1)
        row = (posc // 64).astype(f32)
        col = (posc % 64).astype(f32)
        ang_r = (row[:, None] * inv_freq[None, :]).astype(f32)
        ang_c = (col[:, None] * inv_freq[None, :]).astype(f32)
        C = np.zeros((128, TT), f32)
        Sg = np.zeros((128, TT), f32)
        for p in range(128):
            d = p % 64
            ang = ang_r if d < 32 else ang_c
            dd = d % 32
            f = dd % 16
            C[p] = np.cos(ang[:, f])
            Sg[p] = -np.sin(ang[:, f]) if dd < 16 else np.sin(ang[:, f])
        tabs.append(np.concatenate([C, Sg], 1))
        vms.append(np.ascontiguousarray(np.broadcast_to(valid.astype(f32)[None, :], (128, TT))))
        vp = 1.0 if base - 128 >= 0 else 0.0
        vn = 1.0 if base + TU < L else 0.0
        mks.append(np.concatenate([tri_prev, tri_next, tri_prev * vp, tri_next * vn], 1).astype(ml_dtypes.bfloat16))
        ic = np.zeros((4, 16), f32)
        for gi, w in enumerate((2, 4, 8, 16)):
            for j in range(16):
                t = base + (j if j < 8 else 1016 + (j - 8))
                lo = min(max(t - w // 2, 0), L)
                hi = min(max(t - w // 2 + w, 0), L)
                ic[gi, j] = 1.0 / float(hi - lo)
        invcs.append(np.ascontiguousarray(np.broadcast_to(ic.reshape(1, 64), (128, 64))))
    d = {}
    d["xT"] = np.stack(xs, 0)
    d["tabs"] = np.stack(tabs, 0)
    d["vmask"] = np.stack(vms, 0)
    d["masks"] = np.stack(mks, 0)
    d["invc"] = np.stack(invcs, 0)
    d["ctxT"] = np.ascontiguousarray(inp["ctx"][b].T.reshape(8, 128, 256).transpose(1, 0, 2)).reshape(128, 2048)
    cc = np.stack([inp["c"][b], inp["c_ctx"]], 1)
    d["cT"] = np.ascontiguousarray(cc.reshape(8, 128, 2).transpose(1, 0, 2)).reshape(128, 16)
    return d


_LAST = {}


def kernel(**inputs):
    inp = {k: np.asarray(v) for k, v in inputs.items()}
    nc = build_program()
    sh = _prep_shared(inp)
    in_maps = []
    for c in range(NCORES):
        m = dict(sh)
        m.update(_prep_core(inp, c))
        in_maps.append(m)
    res = run_bass_kernel_spmd(nc, in_maps, core_ids=list(range(NCORES)))
    _LAST["res"] = res
    out = np.zeros((4, 8192, 1024), np.float32)
    for c in range(NCORES):
        b, half = c // 2, c % 2
        o = np.asarray(res.results[c]["outT"]).reshape(NU, 128, 8, TU)
        for u in range(NU):
            base = half * 4096 + u * TU
            out[b, base:base + TU, :] = o[u].transpose(2, 1, 0).reshape(TU, 1024)
    return out
```

```python
import os
import numpy as np
import ml_dtypes
import concourse.bass as bass
import concourse.mybir as mybir
from concourse.bass_utils import run_bass_kernel_spmd

F32, BF16, U8 = mybir.dt.float32, mybir.dt.bfloat16, mybir.dt.uint8
AF = mybir.ActivationFunctionType
ALU = mybir.AluOpType
AX = mybir.AxisListType

NCORES = 8
NU = int(os.environ.get("MK_NU", "4"))
NEXP = int(os.environ.get("MK_NEXP", "64"))
DEBUG = int(os.environ.get("MK_DEBUG", "0"))
STOP = int(os.environ.get("MK_STOP", "9"))
TU, HALO, TT, KC = 1024, 128, 1280, 8
EPS = 1e-6
ENGS = ("pe", "act", "dve", "pool", "sp")


class _Rec:
    def __init__(self):
        self.call = None

    def __getattr__(self, name):
        def f(*a, **k):
            self.call = (name, a, k)
        return f


class Sched:
    def __init__(self, nc):
        self.nc = nc
        self.ops = {e: [] for e in ENGS}
        self.res = {}
        self.dma_count = {}
        self.waited = {e: {} for e in ENGS}
        self.last_compute = {e: -1 for e in ENGS}

    def _res(self, r):
        if r not in self.res:
            self.res[r] = {"w": None, "r": {}}
        return self.res[r]

    def _addwaits(self, eng, deps):
        waits = []
        wd = self.waited[eng]
        for k, i in deps.items():
            if wd.get(k, -1) >= i:
                continue
            wd[k] = i
            waits.append((k, i))
            if k in ENGS:
                self.ops[k][i]["flag"] = True
        return waits

    def op(self, eng, fn, reads=(), writes=(), dma=None):
        if fn is not None:
            rcd = _Rec()
            fn(rcd)
            fn = rcd.call
        lst = self.ops[eng]
        idx = len(lst)
        if dma is not None:
            ck = ("dma", dma)
            cidx = self.dma_count.get(dma, 0) + 1
            self.dma_count[dma] = cidx
        else:
            ck, cidx = eng, idx
        deps = {}

        def add(k, i):
            if deps.get(k, -1) < i:
                deps[k] = i

        for r in reads:
            st = self._res(r)
            if st["w"] is not None:
                add(*st["w"])
        for w in writes:
            st = self._res(w)
            if st["w"] is not None and not (st["w"][0] == ck and eng == "pe" and dma is None):
                add(*st["w"])
            for k, i in st["r"].items():
                if not (k == ck and eng == "pe" and dma is None):
                    add(k, i)
        rec = {"fn": fn, "waits": self._addwaits(eng, deps), "flag": False, "dma": dma}
        lst.append(rec)
        if dma is None and fn is not None:
            self.last_compute[eng] = idx
        for r in reads:
            st = self._res(r)
            if st["r"].get(ck, -1) < cidx:
                st["r"][ck] = cidx
        for w in writes:
            st = self._res(w)
            st["w"] = (ck, cidx)
            st["r"] = {}
        return rec

    def barrier(self):
        last = dict(self.last_compute)
        for e in ENGS:
            deps = {}
            for k in ENGS:
                if k != e and last[k] >= 0:
                    deps[k] = last[k]
            for d, c in self.dma_count.items():
                deps[("dma", d)] = c
            self.ops[e].append({"fn": None, "waits": self._addwaits(e, deps), "flag": False, "dma": None})

    def emit(self):
        nc = self.nc
        for e in ENGS:
            c = 0
            for o in self.ops[e]:
                if o["flag"]:
                    assert o["dma"] is None and o["fn"] is not None
                    c += 1
                    o["val"] = c
        from contextlib import ExitStack
        sems = {}
        with ExitStack() as es:
            for e in ENGS:
                sems[e] = es.enter_context(nc.semaphore("c_" + e))
            for d in self.dma_count:
                sems[("dma", d)] = es.enter_context(nc.semaphore("d_" + d))
            block = es.enter_context(nc.Block())

            def run(e, engobj):
                for o in self.ops[e]:
                    for k, i in o["waits"]:
                        v = self.ops[k][i]["val"] if k in ENGS else 16 * i
                        if v > 0:
                            engobj.wait_ge(sems[k], v)
                    if o["fn"] is None:
                        continue
                    name, a, k = o["fn"]
                    ins = getattr(engobj, name)(*a, **k)
                    if o["dma"] is not None:
                        ins.then_inc(sems[("dma", o["dma"])], 16)
                    elif o["flag"]:
                        ins.then_inc(sems[e], 1)

            @block.tensor
            def _(pe):
                run("pe", pe)

            @block.scalar
            def _(act):
                run("act", act)

            @block.vector
            def _(dve):
                run("dve", dve)

            @block.gpsimd
            def _(pool):
                run("pool", pool)

            @block.sync
            def _(sp):
                run("sp", sp)


def build_program():
    nc = bass.Bass("TRN2", target_bir_lowering=False)
    S = Sched(nc)

    def din(name, shape, dt=F32):
        return nc.dram_tensor(name, list(shape), dt, kind="ExternalInput").ap()

    d_x = din("xT", [NU, 128, KC * TT])
    d_tab = din("tabs", [NU, 128, 2 * TT])
    d_vm = din("vmask", [NU, 128, TT])
    d_mk = din("masks", [NU, 128, 4 * 512], BF16)
    d_invc = din("invc", [NU, 128, 64])
    d_ctx = din("ctxT", [128, KC * 256])
    d_c = din("cT", [128, KC * 2])
    d_wada = din("w_ada", [6, 128, KC * 1024])
    d_bada = din("b_adaT", [128, 48])
    d_gvec = din("gvec", [128, 24])
    d_win = din("w_in", [128, KC * 1920])
    d_wout = din("w_out", [128, KC * 1024])
    d_poolw = din("pool_w", [128, 512])
    d_pscale = din("pool_scale", [128, 4])
    d_wr = din("w_router", [128, KC * 64])
    d_rb = din("rbias", [128, 64])
    d_sink = din("sink", [128, 8])
    if STOP > 4:
        d_wg = din("wg", [65, 128, 2048])
        d_wu = din("wu", [65, 128, 2048])
        d_wd = din("wd", [65, 128, 2048])
    d_ident = din("ident", [128, 128])
    d_out = nc.dram_tensor("outT", [NU, 128, KC * TU], F32, kind="ExternalOutput").ap()
    dbg = {}
    if DEBUG:
        def dout(name, shape, dt=F32):
            dbg[name] = nc.dram_tensor(name, list(shape), dt, kind="ExternalOutput").ap()
        dout("dbg_mod", [128, 96])
        dout("dbg_q", [128, 4 * TT], BF16)
        dout("dbg_k", [128, TT], BF16)
        dout("dbg_v", [128, 10 * 130], BF16)
        dout("dbg_p", [128, 4 * TT])
        dout("dbg_mix", [128, 8 * TU], BF16)
        dout("dbg_x1", [128, KC * TT])
        dout("dbg_h2", [128, 8 * TU], BF16)
        dout("dbg_g", [64, TU])

    ARENA = 208896
    arena = nc.alloc_sbuf_tensor("arena", [128, ARENA], U8)[:]
    cur = [0]

    def alloc(nbytes):
        off = cur[0]
        cur[0] += (nbytes + 63) // 64 * 64
        assert cur[0] <= ARENA, cur[0]
        return off

    def V(off, n, dt, parts=128, p0=0):
        esz = 4 if dt == F32 else 2
        return arena[p0:p0 + parts, off:off + n * esz].bitcast(dt)

    def V3(off, a, b, dt):
        return V(off, a * b, dt).rearrange("p (a b) -> p a b", a=a)

    o_cst = alloc(16 * 8 * 4)
    cst = V3(o_cst, 16, 8, F32)
    o_gvec = alloc(24 * 4); gvec = V3(o_gvec, 3, 8, F32)
    o_mod = alloc(96 * 4); modS = V3(o_mod, 48, 2, F32)
    o_bada = alloc(48 * 4); badaT = V(o_bada, 48, F32)
    o_psc = alloc(16); pscale = V(o_psc, 4, F32)
    o_esink = alloc(32); esink = V(o_esink, 8, F32)
    o_rb = alloc(256); rbias = V(o_rb, 64, F32)
    o_ones = alloc(256); onesb = V(o_ones, 128, BF16)
    o_idf = alloc(512); identf = V(o_idf, 128, F32)
    o_idb = alloc(256); identb = V(o_idb, 128, BF16)
    o_ct = alloc(64); cTt = V3(o_ct, 8, 2, F32)
    o_sct = alloc(64); scT = V3(o_sct, 8, 2, F32)
    o_wr = alloc(KC * 64 * 4); wr = V3(o_wr, 8, 64, F32)
    o_poolw = alloc(512 * 2); poolw = V3(o_poolw, 4, 128, BF16)
    o_kc = alloc(256 * 2); KcT = V(o_kc, 256, BF16)
    o_vc = alloc(2 * 130 * 2); Vc = V(o_vc, 260, BF16).rearrange("p (a g d) -> p a g d", a=2, g=2)
    gscr = nc.dram_tensor("gscr", [64, TU], F32).ap()
    o_wout = alloc(KC * 1024 * 2); wout = V3(o_wout, 8, 1024, BF16)
    o_x = alloc(KC * TT * 4); xT = V3(o_x, 8, TT, F32)
    o_xs = alloc(KC * 512 * 4); xstage = V3(o_xs, 8, 512, F32)
    R1 = alloc(30720)
    R3 = alloc(36864)
    R4a = alloc(24576)
    R4b = alloc(32768)
    win = V3(R1, 8, 1920, BF16)
    pt = [V(R1 + i * 1024, 512, BF16) for i in range(10)]
    attn_tok = [V(R1 + 10240 + i * 1024, 512, BF16) for i in range(2)]
    tA = V(R1 + 12288, TT, F32)
    tB = V(R1 + 12288 + 5120, TT, F32)
    dF = V(R1 + 22528, TU, F32)
    dTb = V(R1 + 26624, TU, BF16)
    denb = V(R1 + 28672, 8, F32)
    recb = V(R1 + 28736, 8, F32)
    tmpE = V(R1 + 28800, 16, F32)
    QT = V3(R3, 4, TT, BF16)
    KT = V(R3 + 10240, TT, BF16)
    Vtok = V(R3 + 12800, 10 * 130, BF16).rearrange("p (a g d) -> p a g d", a=10, g=2)
    pT = V3(R3 + 15424, 4, TT, F32)
    h2T = V3(R3, 8, TU, BF16)
    gT = V(R3 + 16384, TU, F32, parts=64)
    rt = {}
    ro = R3 + 20480
    for nm in ("sc", "bi", "ta", "tb", "msk", "gate"):
        rt[nm] = V(ro, 512, F32); ro += 2048
    for nm in ("m1", "m2", "gs", "t8g", "gm", "t8e"):
        rt[nm] = V(ro, 64, F32); ro += 256
    for nm in ("den", "rden"):
        rt[nm] = V(ro, 8, F32); ro += 64
    assert ro <= R3 + 36864
    Wgu = [V(R4a + s * 12288, 4096, BF16) for s in range(2)]
    Wd = [V(R4a + s * 12288 + 8192, 2048, BF16) for s in range(2)]
    sqA = V3(R4a, 8, 512, BF16)
    hT = [V3(R4a + 8192 + i * 8192, 8, 512, BF16) for i in range(2)]
    tabC = V(R4b, TT, F32)
    tabS = V(R4b + 5120, TT, F32)
    vmask = V(R4b + 10240, TT, F32)
    rstdA = V(R4b + 15360, 512, F32)
    xnA = [V(R4b + 17408 + i * 2048, 512, F32) for i in range(2)]
    rtmp = [V(R4b + 21504 + i * 2048, 512, F32) for i in range(2)]
    mk = V3(R4b + 25600, 4, 512, BF16)
    invc = V3(R4b + 29696, 4, 16, F32)
    mixT = V3(R4b + 4608, 8, TU, BF16)
    sqD = V3(R4b, 8, 512, BF16)
    rstdD = V(R4b + 8192, 512, F32)
    xnD = [V(R4b + 10240 + i * 2048, 512, F32) for i in range(2)]
    h2f = V3(R4b + 14336, 8, 512, F32)
    sgb = [[V(R4b + (p * 2 + f) * 2048, 512, F32) for f in range(2)] for p in range(2)]
    tbuf = [V(R4b + 8192 + i * 2048, 512, F32) for i in range(2)]
    gbs = [V(R4b + 12288 + i * 2048, 512, F32) for i in range(4)]
    ATb = [V3(R4b + 20480 + i * 2048, 2, 512, BF16) for i in range(2)]
    sqF = V3(R4b, 8, 512, BF16)
    rstdF = V(R4b + 8192, 512, F32)
    outT = V3(R4b + 10240, 8, 512, F32)
    wa = [V3(R1 + i * 32768, 8, 1024, F32) for i in range(3)]
    hc = V3(R4a, 8, 256, BF16)
    ctxT = V3(R4b, 8, 256, F32)
    sqC = V3(R4b + 8192, 8, 256, BF16)
    rstdC = V(R4b + 12288, 256, F32)
    xnC = [V(R4b + 13312 + i * 1024, 256, F32) for i in range(2)]

    pb = [nc.alloc_psum_tensor("pb%d" % i, [128, 512], F32)[:] for i in range(7)]
    pbt = nc.alloc_psum_tensor("pbt", [128, 1024], BF16)[:]
    pb7 = pbt.bitcast(F32)

    def dma(eng, out, in_, writes=(), reads=(), sem=None):
        S.op(eng, lambda e: e.dma_start(out=out, in_=in_), reads=reads, writes=writes, dma=sem)

    dma("sp", cTt.rearrange("p a b -> p (a b)"), d_c, ["cT"], sem="ld_c")
    dma("sp", badaT, d_bada, ["bada"], sem="ld_bada")
    dma("sp", gvec.rearrange("p a b -> p (a b)"), d_gvec, ["gvec"], sem="ld_gvec")
    dma("sp", pscale, d_pscale, ["pscale"], sem="ld_psc")
    dma("sp", rbias, d_rb, ["rbias"], sem="ld_rb")
    dma("sp", esink, d_sink, ["esink"], sem="ld_sink")
    dma("sp", identf, d_ident, ["identf"], sem="ld_id")
    dma("pool", identb, d_ident, ["identb"], sem="ld_idb")
    dma("sp", wr.rearrange("p a b -> p (a b)"), d_wr, ["wr"], sem="ld_wr")
    dma("pool", poolw.rearrange("p a b -> p (a b)"), d_poolw, ["poolw"], sem="ld_pw")
    for k in range(8):
        dma("pool", wout[:, k, :], d_wout[:, k * 1024:(k + 1) * 1024], ["wout"], sem="ld_wout")
    S.op("pool", lambda e: e.memset(onesb, 1.0), writes=["onesb"])
    S.op("pool", lambda e: e.memset(Vc[:, :, :, 64:65], 1.0), writes=["Vc1"])
    S.op("act", lambda e: e.activation(out=scT, in_=cTt, func=AF.Silu), reads=["cT"], writes=["scT"])
    S.op("act", lambda e: e.activation(out=esink, in_=esink, func=AF.Exp), reads=["esink"], writes=["esink"])
    psmod = pb[0][:, 0:96].rearrange("p (a b) -> p a b", b=2)
    for v in range(6):
        b = v % 3
        dma("sp", wa[b].rearrange("p a b -> p (a b)"), d_wada[v], ["wa%d" % b], sem="ld_wa%d" % b)
        for m in range(8):
            for k in range(8):
                S.op("pe", lambda e, b=b, m=m, k=k, v=v: e.matmul(
                    psmod[:, v * 8 + m, :], wa[b][:, k, m * 128:(m + 1) * 128], scT[:, k, :],
                    start=(k == 0), stop=(k == 7)), reads=["wa%d" % b, "scT"], writes=["psmod"])
    S.op("dve", lambda e: e.tensor_tensor(out=modS, in0=psmod, in1=badaT.unsqueeze(2).to_broadcast([128, 48, 2]),
                                          op=ALU.add), reads=["psmod", "bada"], writes=["modS"])

    def cst_copy(row, v, col):
        S.op("dve", lambda e: e.tensor_copy(out=cst[:, row, :], in_=modS[:, v * 8:(v + 1) * 8, col]),
             reads=["modS"], writes=["cst"])

    def cst_scale(row, v, col, gi):
        S.op("dve", lambda e: e.scalar_tensor_tensor(out=cst[:, row, :], in0=modS[:, v * 8:(v + 1) * 8, col], scalar=1.0,
                                                     in1=gvec[:, gi, :], op0=ALU.add, op1=ALU.mult),
             reads=["modS", "gvec"], writes=["cst"])

    cst_copy(0, 0, 0); cst_scale(1, 1, 0, 0); cst_copy(2, 2, 0)
    cst_copy(3, 3, 0); cst_scale(4, 4, 0, 1); cst_copy(5, 5, 0)
    cst_copy(6, 0, 1); cst_scale(7, 1, 1, 0)
    S.op("dve", lambda e: e.tensor_copy(out=cst[:, 8, :], in_=gvec[:, 2, :]), reads=["gvec"], writes=["cst"])
    if DEBUG:
        dma("sp", dbg["dbg_mod"], modS.rearrange("p a b -> p (a b)"), reads=["modS"], writes=["o_mod"], sem="st_dbg0")
    S.barrier()

    ss_bank = pb[0]

    def rms_stats(src3, n, sq, rstd, tag, srcn=None):
        srcn = srcn or (tag + "_src")
        S.op("act", lambda e: e.activation(out=sq[:, :, 0:n], in_=src3, func=AF.Square),
             reads=[srcn], writes=[tag + "_sq"])
        for k in range(8):
            S.op("pe", lambda e, k=k: e.matmul(ss_bank[:, 0:n], onesb, sq[:, k, 0:n], start=(k == 0), stop=(k == 7)),
                 reads=[tag + "_sq", "onesb"], writes=["ss_bank"])
        S.op("dve", lambda e: e.tensor_scalar(out=rstd[:, 0:n], in0=ss_bank[:, 0:n], scalar1=1.0 / 1024, scalar2=EPS,
                                              op0=ALU.mult, op1=ALU.add), reads=["ss_bank"], writes=[tag + "_rstd"])
        S.op("act", lambda e: e.activation(out=rstd[:, 0:n], in_=rstd[:, 0:n], func=AF.Sqrt),
             reads=[tag + "_rstd"], writes=[tag + "_rstd"])
        S.op("dve", lambda e: e.reciprocal(out=rstd[:, 0:n], in_=rstd[:, 0:n]), reads=[tag + "_rstd"], writes=[tag + "_rstd"])

    def modulate(src3, n, rstd, xn, arow, brow, dst3, tag, dst_name, srcn=None):
        srcn = srcn or (tag + "_src")
        for k in range(8):
            b = k % 2
            S.op("dve", lambda e, k=k, b=b: e.tensor_tensor(out=xn[b][:, 0:n], in0=src3[:, k, :], in1=rstd[:, 0:n], op=ALU.mult),
                 reads=[srcn, tag + "_rstd"], writes=[tag + "_xn%d" % b])
            S.op("act", lambda e, k=k, b=b: e.activation(out=dst3[:, k, :], in_=xn[b][:, 0:n], func=AF.Identity,
                                                         bias=cst[:, brow, k:k + 1], scale=cst[:, arow, k:k + 1]),
                 reads=[tag + "_xn%d" % b, "cst"], writes=[dst_name])

    def load_win():
        for k in range(8):
            dma("pool", win[:, k, :], d_win[:, k * 1920:(k + 1) * 1920], ["win"], sem="ld_win")

    load_win()
    dma("sp", ctxT.rearrange("p a b -> p (a b)"), d_ctx, ["C_src"], sem="ld_ctx")
    rms_stats(ctxT, 256, sqC, rstdC, "C")
    modulate(ctxT, 256, rstdC, xnC, 7, 6, hc, "C", "hc")
    OC_K, OC_KP, OC_P, OC_V = 8, 9, 10, 14
    for k in range(8):
        S.op("pe", lambda e, k=k: e.matmul(pb[1][:, 0:256], win[:, k, OC_K * 128:(OC_K + 1) * 128], hc[:, k, :],
                                           start=(k == 0), stop=(k == 7)), reads=["win", "hc"], writes=["pb1"])
    S.op("act", lambda e: e.copy(out=KcT, in_=pb[1][:, 0:256]), reads=["pb1"], writes=["KcT"])
    for cb in range(2):
        for k in range(8):
            S.op("pe", lambda e, k=k, cb=cb: e.matmul(pb[2][:, cb * 128:(cb + 1) * 128], hc[:, k, cb * 128:(cb + 1) * 128],
                                                      win[:, k, OC_V * 128:(OC_V + 1) * 128], start=(k == 0), stop=(k == 7)),
                 reads=["win", "hc"], writes=["pb2"])
        S.op("act", lambda e, cb=cb: e.copy(out=Vc[:, cb, :, 0:64],
                                            in_=pb[2][:, cb * 128:(cb + 1) * 128].rearrange("p (g d) -> p g d", g=2)),
             reads=["pb2"], writes=["Vc"])
    S.barrier()

    CH = [(0, 512), (512, 512), (1024, 256)]
    if STOP <= 0:
        NUL = 0
    else:
        NUL = NU

    def w_dma_gu(e_idx, s):
        dma("pool", Wgu[s][:, 0:2048], d_wg[e_idx], ["Wgu%d" % s], sem="ld_wg%d" % s)
        dma("pool", Wgu[s][:, 2048:4096], d_wu[e_idx], ["Wgu%d" % s], sem="ld_wu%d" % s)

    def w_dma_d(e_idx, s):
        dma("pool", Wd[s], d_wd[e_idx], ["Wd%d" % s], sem="ld_wd%d" % s)

    def w_dma(e_idx, s):
        w_dma_gu(e_idx, s)
        w_dma_d(e_idx, s)

    elist = list(range(NEXP)) + [64]

    for u in range(NUL):
        dx3 = d_x[u].rearrange("p (a b) -> p a b", a=8)
        for ci, (c0, n) in enumerate(CH):
            if ci == 0 and u > 0:
                S.op("pool", lambda e: e.tensor_copy(out=xT[:, :, 0:512], in_=xstage), reads=["xs"], writes=["xT"])
                continue
            dma("sp", xT[:, :, c0:c0 + n], dx3[:, :, c0:c0 + n], ["A%d_src" % ci, "xT"], sem="ld_x%d" % ci)
        dma("sp", mk.rearrange("p a b -> p (a b)"), d_mk[u], ["mk"], sem="ld_mk")
        dma("sp", invc.rearrange("p a b -> p (a b)"), d_invc[u], ["invc"], sem="ld_invc")
        dma("sp", tabC, d_tab[u][:, 0:TT], ["tabC"], sem="ld_tc")
        dma("sp", tabS, d_tab[u][:, TT:2 * TT], ["tabS"], sem="ld_ts")
        dma("sp", vmask, d_vm[u], ["vmask"], sem="ld_vm")
        S.op("pool", lambda e: e.memset(Vtok[:, :, :, 64:65], 1.0), writes=["Vtok1"])
        def normA(ci):
            c0, n = CH[ci]
            hb = hT[ci % 2]
            hn = "hT%d" % (ci % 2)
            src3 = xT[:, :, c0:c0 + n]
            srcn = "A%d_src" % ci
            if ci == 0 and u > 0:
                src3, srcn = xstage, "xs"
            rms_stats(src3, n, sqA, rstdA, "A", srcn)
            modulate(src3, n, rstdA, xnA, 1, 0, hb[:, :, 0:n], "A", hn, srcn)

        def projA(ci):
            c0, n = CH[ci]
            hb = hT[ci % 2]
            hn = "hT%d" % (ci % 2)

            def proj(bank, oc, bname):
                for k in range(8):
                    S.op("pe", lambda e, k=k: e.matmul(pb[bank][:, 0:n], win[:, k, oc * 128:(oc + 1) * 128], hb[:, k, 0:n],
                                                       start=(k == 0), stop=(k == 7)), reads=["win", hn], writes=[bname])

            for pi in range(5):
                ba, bb = (1, 2) if pi % 2 == 0 else (3, 4)
                oc_a, oc_b = (pi, 4 + pi) if pi < 4 else (OC_K, OC_KP)
                proj(ba, oc_a, "pb%d" % ba)
                proj(bb, oc_b, "pb%d" % bb)
                dst = QT[:, pi, c0:c0 + n] if pi < 4 else KT[:, c0:c0 + n]
                dname = "QT" if pi < 4 else "KT"
                S.op("dve", lambda e, ba=ba: e.tensor_tensor(out=rtmp[0][:, 0:n], in0=pb[ba][:, 0:n], in1=tabC[:, c0:c0 + n], op=ALU.mult),
                     reads=["pb%d" % ba, "tabC"], writes=["rtmp0"])
                S.op("dve", lambda e, bb=bb: e.tensor_tensor(out=rtmp[1][:, 0:n], in0=pb[bb][:, 0:n], in1=tabS[:, c0:c0 + n], op=ALU.mult),
                     reads=["pb%d" % bb, "tabS"], writes=["rtmp1"])
                S.op("dve", lambda e, dst=dst: e.tensor_tensor(out=dst, in0=rtmp[0][:, 0:n], in1=rtmp[1][:, 0:n], op=ALU.add),
                     reads=["rtmp0", "rtmp1"], writes=[dname])
            for g in range(4):
                bk = 5
                proj(bk, OC_P + g, "pb%d" % bk)
                S.op("dve", lambda e, g=g, bk=bk: e.tensor_tensor(out=pT[:, g, c0:c0 + n], in0=pb[bk][:, 0:n], in1=vmask[:, c0:c0 + n], op=ALU.mult),
                     reads=["pb%d" % bk, "vmask"], writes=["pT"])
            for j in range(n // 128):
                blk = c0 // 128 + j
                reg = pb[6][:, (j % 2) * 128:(j % 2) * 128 + 128]
                rn = "pb6"
                for k in range(8):
                    S.op("pe", lambda e, k=k, j=j, reg=reg: e.matmul(reg, hb[:, k, j * 128:(j + 1) * 128], win[:, k, OC_V * 128:(OC_V + 1) * 128],
                                                                     start=(k == 0), stop=(k == 7)), reads=["win", hn], writes=[rn])
                S.op("act", lambda e, blk=blk, reg=reg: e.copy(out=Vtok[:, blk, :, 0:64], in_=reg.rearrange("p (g d) -> p g d", g=2)),
                     reads=[rn], writes=["Vtok"])

        normA(0)
        normA(1)
        projA(0)
        normA(2)
        projA(1)
        projA(2)
        if DEBUG and u == 0:
            S.barrier()
            dma("sp", dbg["dbg_q"], QT.rearrange("p a b -> p (a b)"), reads=["QT"], writes=["o1"], sem="st_dbg1")
            dma("sp", dbg["dbg_k"], KT, reads=["KT"], writes=["o2"], sem="st_dbg2")
            dma("sp", dbg["dbg_v"], Vtok.rearrange("p a g d -> p (a g d)"), reads=["Vtok"], writes=["o3"], sem="st_dbg3")
            dma("sp", dbg["dbg_p"], pT.rearrange("p a b -> p (a b)"), reads=["pT"], writes=["o4"], sem="st_dbg4")
        S.barrier()
        if STOP <= 1:
            break
        for i in range(min(2, len(elist)) if STOP > 4 else 0):
            w_dma(elist[i], i)
        def pool_group(gi):
            w = 2 ** (gi + 1)
            p = pT[:, gi, :]
            S.op("pool", lambda e, p=p: e.tensor_tensor(out=tA[:, 1:TT], in0=p[:, 0:TT - 1], in1=p[:, 1:TT], op=ALU.add),
                 reads=["pT"], writes=["tA"])
            Sb, sname = tA, "tA"
            if gi >= 1:
                S.op("pool", lambda e: e.tensor_tensor(out=tB[:, 2:1279], in0=tA[:, 1:1278], in1=tA[:, 3:1280], op=ALU.add),
                     reads=["tA"], writes=["tB"])
                Sb, sname = tB, "tB"
            if gi >= 2:
                S.op("pool", lambda e: e.tensor_tensor(out=tA[:, 4:1277], in0=tB[:, 2:1275], in1=tB[:, 6:1279], op=ALU.add),
                     reads=["tB"], writes=["tA"])
                Sb, sname = tA, "tA"
            if gi >= 3:
                S.op("pool", lambda e: e.tensor_tensor(out=tB[:, 8:1273], in0=tA[:, 4:1269], in1=tA[:, 12:1277], op=ALU.add),
                     reads=["tA"], writes=["tB"])
                Sb, sname = tB, "tB"
            S.op("dve", lambda e, Sb=Sb, p=p, w=w: e.scalar_tensor_tensor(out=dF, in0=Sb[:, 128:1152], scalar=1.0 / w, in1=p[:, 128:1152],
                                                                          op0=ALU.mult, op1=ALU.subtract), reads=[sname, "pT"], writes=["dF"])
            for (eo, so) in ((0, 0), (1016, 8)):
                S.op("dve", lambda e, Sb=Sb, eo=eo, so=so, gi=gi: e.tensor_tensor(out=tmpE[:, so:so + 8], in0=Sb[:, 128 + eo:136 + eo],
                                                                                  in1=invc[:, gi, so:so + 8], op=ALU.mult),
                     reads=[sname, "invc"], writes=["tmpE%d" % so])
                S.op("dve", lambda e, p=p, eo=eo, so=so: e.tensor_tensor(out=dF[:, eo:eo + 8], in0=tmpE[:, so:so + 8], in1=p[:, 128 + eo:136 + eo],
                                                                         op=ALU.subtract), reads=["tmpE%d" % so, "pT", "dF"], writes=["dF"])
            S.op("act", lambda e: e.copy(out=dTb, in_=dF), reads=["dF"], writes=["dTb"])
            for tc in range(2):
                bank = 1 + (gi * 2 + tc) % 3
                S.op("pe", lambda e, bank=bank, gi=gi, tc=tc: e.matmul(pb[bank], poolw[:, gi, :], dTb[:, tc * 512:(tc + 1) * 512], start=True, stop=True),
                     reads=["poolw", "dTb"], writes=["pb%d" % bank])
                S.op("act", lambda e, bank=bank, gi=gi, tc=tc: e.activation(out=mixT[:, 4 + gi, tc * 512:(tc + 1) * 512], in_=pb[bank], func=AF.Identity,
                                                                            scale=pscale[:, gi:gi + 1]), reads=["pb%d" % bank, "pscale"], writes=["mixT"])
        sbank = [0, 1, 2, 3, 6]
        sb_i = [0]
        stages = [(i, g) for i in range(8) for g in range(2)]

        def kbs_of(i):
            return [("l", i), ("l", i + 1), ("l", i + 2), ("c", 0), ("c", 1)]

        def qk_stage(i, g):
            qc0 = 128 + 128 * i
            pr0 = 64 * g
            for kbi, (kind, kb) in enumerate(kbs_of(i)):
                bank = sbank[sb_i[0] % 5]
                sb_i[0] += 1
                lhsT = KT[pr0:pr0 + 64, kb * 128:(kb + 1) * 128] if kind == "l" else KcT[pr0:pr0 + 64, kb * 128:(kb + 1) * 128]
                rhs = QT[pr0:pr0 + 64, :, qc0:qc0 + 128]
                slot = pt[g * 5 + kbi]
                sn = "pt%d" % (g * 5 + kbi)
                S.op("pe", lambda e: e.matmul(pb[bank], lhsT, rhs, start=True, stop=True),
                     reads=["KT", "KcT", "QT"], writes=["pb%d" % bank])
                S.op("act", lambda e: e.activation(out=slot, in_=pb[bank], func=AF.Exp, scale=0.125),
                     reads=["pb%d" % bank], writes=[sn])
                mi = None
                if kbi == 0:
                    mi = 2 if i == 0 else 0
                elif kbi == 2:
                    mi = 3 if i == 7 else 1
                if mi is not None:
                    S.op("dve", lambda e: e.tensor_tensor(out=slot, in0=slot, in1=mk[:, mi, :], op=ALU.mult),
                         reads=[sn, "mk"], writes=[sn])

        def pv_stage(i, g):
            at = attn_tok[i % 2]
            atn = "attn%d" % (i % 2)
            ob = 4 + g
            pso = pb[ob][:, 0:260].rearrange("p (h d) -> p h d", h=4)
            kbs = kbs_of(i)
            for hh in range(4):
                for kbi, (kind, kb) in enumerate(kbs):
                    vx = Vtok[:, kb, g, :] if kind == "l" else Vc[:, kb, g, :]
                    slot = pt[g * 5 + kbi]
                    S.op("pe", lambda e: e.matmul(pso[:, hh, :], slot[:, hh * 128:(hh + 1) * 128], vx, start=(kbi == 0), stop=(kbi == 4)),
                         reads=["pt%d" % (g * 5 + kbi), "Vtok", "Vtok1", "Vc", "Vc1"], writes=["pb%d" % ob])
            S.op("dve", lambda e: e.tensor_tensor(out=denb[:, 4 * g:4 * g + 4], in0=pso[:, :, 64], in1=esink[:, 4 * g:4 * g + 4], op=ALU.add),
                 reads=["pb%d" % ob, "esink"], writes=["den%d" % g])
            S.op("dve", lambda e: e.reciprocal(out=recb[:, 4 * g:4 * g + 4], in_=denb[:, 4 * g:4 * g + 4]),
                 reads=["den%d" % g], writes=["rec%d" % g])
            for hh in range(4):
                h = 4 * g + hh
                S.op("dve", lambda e: e.tensor_scalar(out=at[:, h * 64:(h + 1) * 64], in0=pso[:, hh, 0:64], scalar1=recb[:, h:h + 1], scalar2=None, op0=ALU.mult),
                     reads=["pb%d" % ob, "rec%d" % g], writes=[atn])

        def finish_block(i):
            at = attn_tok[i % 2]
            atn = "attn%d" % (i % 2)
            for c2 in range(4):
                S.op("pe", lambda e: e.transpose(pbt[:, c2 * 128:(c2 + 1) * 128], at[:, c2 * 128:(c2 + 1) * 128], identb),
                     reads=[atn, "identb"], writes=["pbt"])
            S.op("act", lambda e: e.copy(out=mixT[:, 0:4, i * 128:(i + 1) * 128], in_=pbt[:, 0:512].rearrange("p (c q) -> p c q", c=4)),
                 reads=["pbt"], writes=["mixT"])
            if i < 4:
                pool_group(i)

        qk_stage(*stages[0])
        pending = None
        for si_, (i, g) in enumerate(stages):
            if si_ + 1 < len(stages):
                qk_stage(*stages[si_ + 1])
            pv_stage(i, g)
            if pending is not None:
                finish_block(pending)
                pending = None
            if g == 1:
                pending = i
        if pending is not None:
            finish_block(pending)
        if DEBUG and u == 0:
            S.barrier()
            dma("sp", dbg["dbg_mix"], mixT.rearrange("p a b -> p (a b)"), reads=["mixT"], writes=["o5"], sem="st_dbg5")
        S.barrier()
        if STOP <= 2:
            break
        if u + 1 < NU:
            load_win()
        bi_ = 0
        for tc in range(2):
            for m in range(8):
                bank = 1 + bi_ % 3
                bi_ += 1
                for f in range(8):
                    S.op("pe", lambda e, bank=bank, m=m, f=f, tc=tc: e.matmul(pb[bank], wout[:, f, m * 128:(m + 1) * 128], mixT[:, f, tc * 512:(tc + 1) * 512],
                                                                              start=(f == 0), stop=(f == 7)), reads=["wout", "mixT"], writes=["pb%d" % bank])
                xs = xT[:, m, 128 + tc * 512:128 + (tc + 1) * 512]
                S.op("dve", lambda e, bank=bank, m=m, xs=xs: e.scalar_tensor_tensor(out=xs, in0=pb[bank], scalar=cst[:, 2, m:m + 1], in1=xs,
                                                                                    op0=ALU.mult, op1=ALU.add),
                     reads=["pb%d" % bank, "cst", "xT"], writes=["xT", "D_src"])
        if DEBUG and u == 0:
            S.barrier()
            dma("sp", dbg["dbg_x1"], xT.rearrange("p a b -> p (a b)"), reads=["xT"], writes=["o6"], sem="st_dbg6")
        S.barrier()
        if STOP <= 3:
            break
        psr8 = pb[4]
        for tc in range(2):
            src3 = xT[:, :, 128 + tc * 512:128 + (tc + 1) * 512]
            rms_stats(src3, 512, sqD, rstdD, "D")
            modulate(src3, 512, rstdD, xnD, 4, 3, h2f, "D", "h2f")
            S.op("act", lambda e: e.copy(out=h2T[:, :, tc * 512:(tc + 1) * 512], in_=h2f), reads=["h2f"], writes=["h2T"])
            for j in range(4):
                blk = tc * 4 + j
                for k in range(8):
                    S.op("pe", lambda e: e.matmul(psr8[:, blk * 64:(blk + 1) * 64], h2f[:, k, j * 128:(j + 1) * 128], wr[:, k, :],
                                                  start=(k == 0), stop=(k == 7)), reads=["h2f", "wr"], writes=["pb4"])
        R = rt
        v8 = lambda a: a.rearrange("p (j e) -> p j e", j=8)
        v64 = lambda a: a.rearrange("p (g i) -> p g i", i=8)
        g8 = lambda a: a.rearrange("p (j g) -> p j g", j=8)
        S.op("act", lambda e: e.activation(out=R["sc"], in_=psr8, func=AF.Sigmoid), reads=["pb4"], writes=["r_sc"])
        S.op("dve", lambda e: e.tensor_tensor(out=v8(R["bi"]), in0=v8(R["sc"]), in1=rbias.unsqueeze(1).to_broadcast([128, 8, 64]), op=ALU.add),
             reads=["r_sc", "rbias"], writes=["r_bi"])
        S.op("dve", lambda e: e.tensor_reduce(out=R["m1"], in_=v64(R["bi"]), axis=AX.X, op=ALU.max), reads=["r_bi"], writes=["r_m1"])
        S.op("dve", lambda e: e.tensor_tensor(out=v64(R["ta"]), in0=v64(R["bi"]), in1=R["m1"].unsqueeze(2).to_broadcast([128, 64, 8]), op=ALU.is_equal),
             reads=["r_bi", "r_m1"], writes=["r_ta"])
        S.op("dve", lambda e: e.scalar_tensor_tensor(out=R["tb"], in0=R["ta"], scalar=-1e9, in1=R["bi"], op0=ALU.mult, op1=ALU.add),
             reads=["r_ta", "r_bi"], writes=["r_tb"])
        S.op("dve", lambda e: e.tensor_reduce(out=R["m2"], in_=v64(R["tb"]), axis=AX.X, op=ALU.max), reads=["r_tb"], writes=["r_m2"])
        S.op("dve", lambda e: e.tensor_tensor(out=R["gs"], in0=R["m1"], in1=R["m2"], op=ALU.add), reads=["r_m1", "r_m2"], writes=["r_gs"])
        for j in range(8):
            S.op("dve", lambda e: e.max(out=R["t8g"][:, j * 8:(j + 1) * 8], in_=R["gs"][:, j * 8:(j + 1) * 8]), reads=["r_gs"], writes=["r_t8g"])
        S.op("dve", lambda e: e.tensor_tensor(out=g8(R["gm"]), in0=g8(R["gs"]), in1=g8(R["t8g"])[:, :, 3:4].to_broadcast([128, 8, 8]), op=ALU.is_ge),
             reads=["r_gs", "r_t8g"], writes=["r_gm"])
        S.op("dve", lambda e: e.scalar_tensor_tensor(out=v64(R["msk"]), in0=v64(R["bi"]), scalar=2.0, in1=R["gm"].unsqueeze(2).to_broadcast([128, 64, 8]),
                                                     op0=ALU.add, op1=ALU.mult), reads=["r_bi", "r_gm"], writes=["r_msk"])
        for j in range(8):
            S.op("dve", lambda e: e.max(out=R["t8e"][:, j * 8:(j + 1) * 8], in_=R["msk"][:, j * 64:(j + 1) * 64]), reads=["r_msk"], writes=["r_t8e"])
        S.op("dve", lambda e: e.tensor_tensor(out=v8(R["ta"]), in0=v8(R["msk"]), in1=g8(R["t8e"])[:, :, 7:8].to_broadcast([128, 8, 64]), op=ALU.is_ge),
             reads=["r_msk", "r_t8e"], writes=["r_ta"])
        S.op("dve", lambda e: e.tensor_tensor(out=R["tb"], in0=R["ta"], in1=R["sc"], op=ALU.mult), reads=["r_ta", "r_sc"], writes=["r_tb"])
        S.op("dve", lambda e: e.tensor_reduce(out=R["den"], in_=v8(R["tb"]), axis=AX.X, op=ALU.add), reads=["r_tb"], writes=["r_den"])
        S.op("dve", lambda e: e.reciprocal(out=R["rden"], in_=R["den"]), reads=["r_den"], writes=["r_rden"])
        S.op("dve", lambda e: e.scalar_tensor_tensor(out=v8(R["gate"]), in0=v8(R["tb"]), scalar=2.5, in1=R["rden"].unsqueeze(2).to_broadcast([128, 8, 64]),
                                                     op0=ALU.mult, op1=ALU.mult), reads=["r_tb", "r_rden"], writes=["r_gate"])
        for hb_ in range(2):
            tb_ = pb[6] if hb_ == 0 else pb[5]
            tn_ = "pb6" if hb_ == 0 else "pb5"
            for j in range(4):
                blk = hb_ * 4 + j
                S.op("pe", lambda e: e.transpose(tb_[0:64, j * 128:(j + 1) * 128], R["gate"][:, blk * 64:(blk + 1) * 64], identf),
                     reads=["r_gate", "identf"], writes=[tn_])
            S.op("act", lambda e: e.copy(out=gT[:, hb_ * 512:(hb_ + 1) * 512], in_=tb_[0:64, :]), reads=[tn_], writes=["gT"])
        dma("sp", gscr, gT, reads=["gT"], writes=["gscr"], sem="st_gs")
        if DEBUG and u == 0:
            S.barrier()
            dma("sp", dbg["dbg_h2"], h2T.rearrange("p a b -> p (a b)"), reads=["h2T"], writes=["o7"], sem="st_dbg7")
            dma("sp", dbg["dbg_g"], gT, reads=["gT"], writes=["o8"], sem="st_dbg8")
        S.barrier()
        if STOP <= 4:
            break
        steps = [(ei, tc) for ei in range(len(elist)) for tc in range(2)]
        ybank = [0, 6]
        yb_i = [0]

        def rec_gu(si, f, which):
            ei, tc = steps[si]
            s = ei % 2
            bank = 1 + f * 2 + which
            woff = which * 2048
            for k in range(8):
                S.op("pe", lambda e, k=k: e.matmul(pb[bank], Wgu[s][:, woff + k * 256 + f * 128: woff + k * 256 + (f + 1) * 128],
                                                   h2T[:, k, tc * 512:(tc + 1) * 512], start=(k == 0), stop=(k == 7)),
                     reads=["Wgu%d" % s, "h2T"], writes=["pb%d" % bank])

        def rec_evac(si, f):
            ei, tc = steps[si]
            par = si % 2
            e_id = elist[ei]
            gbank, ubank = 1 + f * 2, 2 + f * 2
            sg = sgb[par][f]
            sgn = "sg%d%d" % (par, f)
            S.op("act", lambda e: e.activation(out=sg, in_=pb[gbank], func=AF.Silu), reads=["pb%d" % gbank], writes=[sgn])
            at = ATb[par][:, f, :]
            atn = "AT%d" % par
            if e_id == 64:
                S.op("dve", lambda e: e.tensor_tensor(out=at, in0=pb[ubank], in1=sg, op=ALU.mult), reads=["pb%d" % ubank, sgn], writes=[atn])
            else:
                tb = tbuf[f]
                S.op("dve", lambda e: e.tensor_tensor(out=tb, in0=pb[ubank], in1=gbs[si % 4], op=ALU.mult),
                     reads=["pb%d" % ubank, "gbs%d" % (si % 4)], writes=["tb%d" % f])
                S.op("dve", lambda e: e.tensor_tensor(out=at, in0=tb, in1=sg, op=ALU.mult), reads=["tb%d" % f, sgn], writes=[atn])

        def rec_gb(si):
            ei, tc = steps[si]
            e_id = elist[ei]
            if e_id == 64:
                return
            sl = si % 4
            dma("sp", gbs[sl], gscr[e_id:e_id + 1, tc * 512:(tc + 1) * 512].partition_broadcast(128),
                reads=["gscr"], writes=["gbs%d" % sl], sem="ld_gb%d" % sl)

        def rec_d(si, m):
            ei, tc = steps[si]
            s = ei % 2
            par = si % 2
            bank = ybank[yb_i[0] % 2]
            yb_i[0] += 1
            for f in range(2):
                S.op("pe", lambda e, f=f: e.matmul(pb[bank], Wd[s][:, f * 1024 + m * 128: f * 1024 + (m + 1) * 128], ATb[par][:, f, :],
                                                   start=(f == 0), stop=(f == 1)), reads=["Wd%d" % s, "AT%d" % par], writes=["pb%d" % bank])
            xs = xT[:, m, 128 + tc * 512:128 + (tc + 1) * 512]
            S.op("dve", lambda e: e.scalar_tensor_tensor(out=xs, in0=pb[bank], scalar=cst[:, 5, m:m + 1], in1=xs, op0=ALU.mult, op1=ALU.add),
                 reads=["pb%d" % bank, "cst", "xT"], writes=["xT"])

        nst = len(steps)
        if u + 1 < NU:
            dma("sp", xstage, d_x[u + 1].rearrange("p (a b) -> p a b", a=8)[:, :, 0:512], ["xs"], sem="ld_xs")
        for si in range(min(3, nst)):
            rec_gb(si)
        for si in range(nst + 1):
            if si + 3 < nst:
                rec_gb(si + 3)
            if si < nst:
                ei, tc = steps[si]
                if tc == 0 and ei >= 1 and ei + 1 < len(elist):
                    w_dma_gu(elist[ei + 1], (ei + 1) % 2)
            for f in range(2):
                for which in range(2):
                    if si < nst:
                        rec_gu(si, f, which)
                        if which == 1:
                            rec_evac(si, f)
                    if si >= 1:
                        mbase = (f * 2 + which) * 2
                        rec_d(si - 1, mbase)
                        rec_d(si - 1, mbase + 1)
            if si < nst:
                ei, tc = steps[si]
                if tc == 0 and ei >= 1 and ei + 1 < len(elist):
                    w_dma_d(elist[ei + 1], (ei + 1) % 2)
        S.barrier()
        for tc in range(2):
            src3 = xT[:, :, 128 + tc * 512:128 + (tc + 1) * 512]
            S.op("act", lambda e: e.activation(out=sqF, in_=src3, func=AF.Square),
                 reads=["xT"], writes=["F_sq"])
            for k in range(8):
                S.op("pe", lambda e, k=k: e.matmul(ss_bank, onesb, sqF[:, k, :], start=(k == 0), stop=(k == 7)),
                     reads=["F_sq", "onesb"], writes=["ss_bank"])
            S.op("dve", lambda e: e.tensor_scalar(out=rstdF, in0=ss_bank, scalar1=1.0 / 1024, scalar2=EPS, op0=ALU.mult, op1=ALU.add),
                 reads=["ss_bank"], writes=["F_rstd"])
            S.op("act", lambda e: e.activation(out=rstdF, in_=rstdF, func=AF.Sqrt), reads=["F_rstd"], writes=["F_rstd"])
            S.op("dve", lambda e: e.reciprocal(out=rstdF, in_=rstdF), reads=["F_rstd"], writes=["F_rstd"])
            for k in range(8):
                S.op("dve", lambda e, k=k: e.scalar_tensor_tensor(out=outT[:, k, :], in0=src3[:, k, :], scalar=cst[:, 8, k:k + 1], in1=rstdF,
                                                                  op0=ALU.mult, op1=ALU.mult), reads=["xT", "cst", "F_rstd"], writes=["outT"])
            dma("sp", d_out[u].rearrange("p (a b) -> p a b", a=8)[:, :, tc * 512:(tc + 1) * 512], outT, reads=["outT"], writes=["o_out"], sem="st_out")
        S.barrier()

    S.emit()
    return nc


def _partner():
    pi = np.zeros(64, np.int64)
    for d in range(64):
        dd = d % 32
        pi[d] = d + 16 if dd < 16 else d - 16
    return pi


def _prep_shared(inp):
    f32 = np.float32
    sh = {}
    sh["w_ada"] = np.ascontiguousarray(inp["w_ada"][0].reshape(8, 128, 6, 1024).transpose(2, 1, 0, 3)).reshape(6, 128, 8192)
    sh["b_adaT"] = np.ascontiguousarray(inp["b_ada"][0].reshape(48, 128).T)
    gv = np.stack([inp["norm1_g"][0], inp["norm2_g"][0], inp["final_g"]], 0)
    sh["gvec"] = np.ascontiguousarray(gv.reshape(3, 8, 128).transpose(2, 0, 1)).reshape(128, 24)
    pi = _partner()
    cols = []
    for c in range(4):
        cols += [c * 64 + d for d in range(64)] + [(4 + c) * 64 + d for d in range(64)]
    for c in range(4):
        cols += [c * 64 + pi[d] for d in range(64)] + [(4 + c) * 64 + pi[d] for d in range(64)]
    cols += [512 + i for i in range(128)]
    cols += [512 + h * 64 + pi[d] for h in range(2) for d in range(64)]
    cols += [768 + i for i in range(512)]
    cols += [640 + i for i in range(128)]
    wl = inp["w_in"][0][:, np.array(cols)]
    sh["w_in"] = np.ascontiguousarray(wl.reshape(8, 128, 1920).transpose(1, 0, 2)).reshape(128, 8 * 1920)
    sh["w_out"] = np.ascontiguousarray(inp["w_out"][0].reshape(8, 128, 1024).transpose(1, 0, 2)).reshape(128, 8192)
    sh["pool_w"] = np.ascontiguousarray(inp["pool_w"][0].transpose(1, 0, 2)).reshape(128, 512)
    sh["pool_scale"] = np.ascontiguousarray(inp["pool_scale"][0].reshape(4, 128).T)
    sh["w_router"] = np.ascontiguousarray(inp["w_router"][0].reshape(8, 128, 64).transpose(1, 0, 2)).reshape(128, 512)
    sh["rbias"] = np.ascontiguousarray(np.broadcast_to(inp["router_bias"][0][None, :], (128, 64))).astype(f32)
    sh["sink"] = np.ascontiguousarray(np.broadcast_to(inp["attn_sink"][0][None, :], (128, 8))).astype(f32)
    sh["ident"] = np.eye(128, dtype=f32)
    if STOP <= 4:
        return sh
    wg = np.concatenate([inp["w_gate"][0], inp["ws_gate"]], 0)
    wu = np.concatenate([inp["w_up"][0], inp["ws_up"]], 0)
    wd = np.concatenate([inp["w_down"][0], inp["ws_down"]], 0)
    sh["wg"] = np.ascontiguousarray(wg.reshape(65, 8, 128, 256).transpose(0, 2, 1, 3)).reshape(65, 128, 2048)
    sh["wu"] = np.ascontiguousarray(wu.reshape(65, 8, 128, 256).transpose(0, 2, 1, 3)).reshape(65, 128, 2048)
    sh["wd"] = np.ascontiguousarray(wd.reshape(65, 2, 128, 1024).transpose(0, 2, 1, 3)).reshape(65, 128, 2048)
    return sh


def _prep_core(inp, c):
    f32 = np.float32
    b, half = c // 2, c % 2
    L = 8192
    x = inp["x"][b]
    xpad = np.zeros((L + 256, 1024), f32)
    xpad[128:128 + L] = x
    inv_freq = (np.float32(10000.0) ** (-np.arange(16, dtype=f32) / np.float32(16))).astype(f32)
    pi = _partner()
    xs, tabs, vms, mks, invcs = [], [], [], [], []
    kk = np.arange(128)[:, None]
    qq = np.arange(128)[None, :]
    tri_prev = np.tile((qq <= kk).astype(f32), (1, 4))
    tri_next = np.tile((kk <= qq).astype(f32), (1, 4))
    for u in range(NU):
        base = half * 4096 + u * TU
        seg = xpad[base:base + TT]
        xs.append(np.ascontiguousarray(seg.T.reshape(8, 128, TT).transpose(1, 0, 2)).reshape(128, 8 * TT))
        pos = base - 128 + np.arange(TT)
        valid = ((pos >= 0) & (pos < L))
        posc = np.clip(pos, 0, L - 1)
        row = (posc // 64).astype(f32)
        col = (posc % 64).astype(f32)
        ang_r = (row[:, None] * inv_freq[None, :]).astype(f32)
        ang_c = (col[:, None] * inv_freq[None, :]).astype(f32)
        C = np.zeros((128, TT), f32)
        Sg = np.zeros((128, TT), f32)
        for p in range(128):
            d = p % 64
            ang = ang_r if d < 32 else ang_c
            dd = d % 32
            f = dd % 16
            C[p] = np.cos(ang[:, f])
            Sg[p] = -np.sin(ang[:, f]) if dd < 16 else np.sin(ang[:, f])
        tabs.append(np.concatenate([C, Sg], 1))
        vms.append(np.ascontiguousarray(np.broadcast_to(valid.astype(f32)[None, :], (128, TT))))
        vp = 1.0 if base - 128 >= 0 else 0.0
        vn = 1.0 if base + TU < L else 0.0
        mks.append(np.concatenate([tri_prev, tri_next, tri_prev * vp, tri_next * vn], 1).astype(ml_dtypes.bfloat16))
        ic = np.zeros((4, 16), f32)
        for gi, w in enumerate((2, 4, 8, 16)):
            for j in range(16):
                t = base + (j if j < 8 else 1016 + (j - 8))
                lo = min(max(t - w // 2, 0), L)
                hi = min(max(t - w // 2 + w, 0), L)
                ic[gi, j] = 1.0 / float(hi - lo)
        invcs.append(np.ascontiguousarray(np.broadcast_to(ic.reshape(1, 64), (128, 64))))
    d = {}
    d["xT"] = np.stack(xs, 0)
    d["tabs"] = np.stack(tabs, 0)
    d["vmask"] = np.stack(vms, 0)
    d["masks"] = np.stack(mks, 0)
    d["invc"] = np.stack(invcs, 0)
    d["ctxT"] = np.ascontiguousarray(inp["ctx"][b].T.reshape(8, 128, 256).transpose(1, 0, 2)).reshape(128, 2048)
    cc = np.stack([inp["c"][b], inp["c_ctx"]], 1)
    d["cT"] = np.ascontiguousarray(cc.reshape(8, 128, 2).transpose(1, 0, 2)).reshape(128, 16)
    return d


_LAST = {}


def kernel(**inputs):
    inp = {k: np.asarray(v) for k, v in inputs.items()}
    nc = build_program()
    sh = _prep_shared(inp)
    in_maps = []
    for c in range(NCORES):
        m = dict(sh)
        m.update(_prep_core(inp, c))
        in_maps.append(m)
    res = run_bass_kernel_spmd(nc, in_maps, core_ids=list(range(NCORES)))
    _LAST["res"] = res
    out = np.zeros((4, 8192, 1024), np.float32)
    for c in range(NCORES):
        b, half = c // 2, c % 2
        o = np.asarray(res.results[c]["outT"]).reshape(NU, 128, 8, TU)
        for u in range(NU):
            base = half * 4096 + u * TU
            out[b, base:base + TU, :] = o[u].transpose(2, 1, 0).reshape(TU, 1024)
    return out
```
